# Optimizing a Trainium2 kernel written in Bass

```python
import math
import jax, jax.numpy as jnp
from jax import lax
import numpy as np

D_MODEL = 1024
BATCH = 4
SEQ = 4096
DEPTH = 1
DEC_BATCH = 32
DEC_SEQ = 8
PAST_LEN = 8192
PAGE_SIZE = 128

N_DA_HEADS = 4
DA_DK = 64
DA_DV = 128
DA_WIDTH = N_DA_HEADS * DA_DV
QK_WIDTH = N_DA_HEADS * 2 * DA_DK
N_SG_GROUPS = 4
SG_CH = (D_MODEL - DA_WIDTH) // N_SG_GROUPS
SG_WIDTH = N_SG_GROUPS * SG_CH
CHUNK = 128
IN_WIDTH = 2 * QK_WIDTH + DA_WIDTH + 2 * SG_WIDTH
MIX_WIDTH = DA_WIDTH + SG_WIDTH
N_BUCKETS = 32
MAX_DISTANCE = 128
Q_BLOCK = 128
N_EXPERT_GROUPS = 4
EXPERTS_PER_GROUP = 4
N_EXPERTS = N_EXPERT_GROUPS * EXPERTS_PER_GROUP
TOP_K_IN_GROUP = 2
D_EXPERT = 512
N_MOD = 6
EPS = 1e-6

kernel_name = "hymba_diffattn_gmlp_hmoe_step"


def rmsnorm(x, g=None):
    xf = x.astype(jnp.float32)
    y = (xf * lax.rsqrt(jnp.mean(xf * xf, axis=-1, keepdims=True) + EPS)).astype(x.dtype)
    return y if g is None else y * g


def layernorm(x, g, b):
    xf = x.astype(jnp.float32)
    mu = jnp.mean(xf, axis=-1, keepdims=True)
    var = jnp.mean(jnp.square(xf - mu), axis=-1, keepdims=True)
    return ((xf - mu) * lax.rsqrt(var + EPS)).astype(x.dtype) * g + b


def t5_bucket(n):
    n = jnp.maximum(n, 0)
    max_exact = N_BUCKETS // 2
    nf = jnp.maximum(n, 1).astype(jnp.float32)
    large = max_exact + (jnp.log(nf / max_exact) / math.log(MAX_DISTANCE / max_exact)
                         * (N_BUCKETS - max_exact)).astype(jnp.int32)
    large = jnp.minimum(large, N_BUCKETS - 1)
    return jnp.where(n < max_exact, n, large)


def diff_attn(q, k, v, q_pos, k_pos, lam, rel_bias):
    s = jnp.einsum('bqhmd,bkhmd->bhmqk', q, k,
                   preferred_element_type=jnp.float32) * (DA_DK ** -0.5)
    dist = q_pos[:, None] - k_pos[None, :]
    bias = jnp.transpose(rel_bias[t5_bucket(dist)], (2, 0, 1))
    s = s + bias[None, :, None].astype(jnp.float32)
    s = jnp.where((dist >= 0)[None, None, None], s, -jnp.inf)
    p = jax.nn.softmax(s, axis=-1)
    a = p[:, :, 0] - lam * p[:, :, 1]
    return jnp.einsum('bhqk,bkhd->bqhd', a.astype(v.dtype), v)


def attend_prompt(q, k, v, lam, rel_bias):
    B, T = q.shape[0], q.shape[1]
    nb = T // Q_BLOCK
    qb = q.reshape(B, nb, Q_BLOCK, N_DA_HEADS, 2, DA_DK).swapaxes(0, 1)
    k_pos = jnp.arange(T)

    def one(args):
        qi, i = args
        return diff_attn(qi, k, v, i * Q_BLOCK + jnp.arange(Q_BLOCK), k_pos, lam, rel_bias)

    o = lax.map(one, (qb, jnp.arange(nb)))
    return o.swapaxes(0, 1).reshape(B, T, N_DA_HEADS, DA_DV)


def attend_sample(q, k, v, past_k, past_v, lam, rel_bias):
    P = past_k.shape[1]
    Tq = q.shape[1]
    k_all = jnp.concatenate([past_k.astype(k.dtype), k], axis=1)
    v_all = jnp.concatenate([past_v.astype(v.dtype), v], axis=1)
    return diff_attn(q, k_all, v_all, P + jnp.arange(Tq), jnp.arange(P + Tq), lam, rel_bias)


def spatial_gate(vs, w_s, b_s):
    B, T, G, C = vs.shape
    nc = -(-T // CHUNK)
    vp = jnp.pad(vs, ((0, 0), (0, nc * CHUNK - T), (0, 0), (0, 0))).reshape(B, nc, CHUNK, G, C)
    s = jnp.einsum('gij,bnjgc->bnigc', jnp.tril(w_s), vp) + b_s.T[None, None, :, :, None]
    return s.reshape(B, nc * CHUNK, G, C)[:, :T]


def mix_tokens(h, attend, p, lam, lam_init):
    B, T, _ = h.shape
    z = h @ p['w_in']
    q, k, v, u, vs = jnp.split(z, [QK_WIDTH, 2 * QK_WIDTH, 2 * QK_WIDTH + DA_WIDTH,
                                   2 * QK_WIDTH + DA_WIDTH + SG_WIDTH], axis=-1)
    q = q.reshape(B, T, N_DA_HEADS, 2, DA_DK)
    k = k.reshape(B, T, N_DA_HEADS, 2, DA_DK)
    v = v.reshape(B, T, N_DA_HEADS, DA_DV)
    o = attend(q, k, v, lam)
    o = rmsnorm(o, p['g_subln']) * (1.0 - lam_init)
    vs = layernorm(vs.reshape(B, T, N_SG_GROUPS, SG_CH), p['g_sg_ln'], p['b_sg_ln'])
    sg = u.reshape(B, T, N_SG_GROUPS, SG_CH) * spatial_gate(vs, p['w_s'], p['b_s'])
    merged = jnp.concatenate([o.reshape(B, T, DA_WIDTH), sg.reshape(B, T, SG_WIDTH)], axis=-1)
    return merged @ p['w_o'], k.reshape(B, T, N_DA_HEADS, 2 * DA_DK), v, vs


def hier_moe(h, p):
    B, T, D = h.shape
    hf = h.reshape(-1, D)
    lg = (hf @ p['w_rg'] + p['b_rg']).astype(jnp.float32)
    pg = jax.nn.softmax(lg, axis=-1)
    gi = jnp.argmax(lg, axis=-1)
    gp = jnp.take_along_axis(pg, gi[:, None], axis=1)[:, 0]
    le = (hf @ p['w_re'] + p['b_re']).astype(jnp.float32).reshape(-1, N_EXPERT_GROUPS, EXPERTS_PER_GROUP)
    sel = jnp.take_along_axis(le, gi[:, None, None], axis=1)[:, 0]
    tv, ti = lax.top_k(sel, TOP_K_IN_GROUP)
    tw = jax.nn.softmax(tv, axis=-1) * gp[:, None]
    eid = gi[:, None] * EXPERTS_PER_GROUP + ti
    comb = jnp.sum(jax.nn.one_hot(eid, N_EXPERTS, dtype=jnp.float32) * tw[..., None], axis=1)
    comb = comb.astype(h.dtype)
    y = jnp.zeros_like(hf)
    for e in range(N_EXPERTS):
        he = jax.nn.silu(hf @ p['w_gate'][e]) * (hf @ p['w_up'][e])
        y = y + comb[:, e:e + 1] * (he @ p['w_down'][e])
    return y.reshape(B, T, D)


def decoder_layer(x, c, attend, p, lam, lam_init):
    mod = (jax.nn.silu(c) @ p['w_ada'] + p['b_ada'])[:, None, :]
    sh1, sc1, g1, sh2, sc2, g2 = jnp.split(mod, N_MOD, axis=-1)
    h = rmsnorm(x) * (1 + sc1) + sh1
    m, k, v, vs = mix_tokens(h, attend, p, lam, lam_init)
    x = x + g1 * m
    h = rmsnorm(x) * (1 + sc2) + sh2
    x = x + g2 * hier_moe(h, p)
    return x, k, v, vs


def setup_inputs(seed: int = 0) -> dict:
    key = jax.random.key(seed)
    ks = jax.random.split(key, 32)
    n_pages = PAST_LEN // PAGE_SIZE
    n_pool = (DEC_BATCH * n_pages * 5) // 4
    f32 = jnp.float32

    def nrm(k, shape, s):
        return jax.random.normal(k, shape, f32) * s

    page_table = jax.random.permutation(ks[6], n_pool)[:DEC_BATCH * n_pages]
    page_table = page_table.reshape(DEC_BATCH, n_pages).astype(jnp.int32)
    return {
        'x_prompt': nrm(ks[0], (BATCH, SEQ, D_MODEL), 1.0),
        'x_sample': nrm(ks[1], (DEC_BATCH, DEC_SEQ, D_MODEL), 1.0),
        'c_prompt': nrm(ks[2], (BATCH, D_MODEL), 1.0),
        'c_sample': nrm(ks[3], (DEC_BATCH, D_MODEL), 1.0),
        'cache_k': nrm(ks[4], (n_pool, DEPTH, PAGE_SIZE, N_DA_HEADS, 2 * DA_DK), 1.0),
        'cache_v': nrm(ks[5], (n_pool, DEPTH, PAGE_SIZE, N_DA_HEADS, DA_DV), 1.0),
        'page_table': page_table,
        'w_ada': nrm(ks[7], (DEPTH, D_MODEL, N_MOD * D_MODEL), 0.5 * D_MODEL ** -0.5),
        'b_ada': nrm(ks[8], (DEPTH, N_MOD * D_MODEL), 0.01),
        'w_in': nrm(ks[9], (DEPTH, D_MODEL, IN_WIDTH), D_MODEL ** -0.5),
        'w_o': nrm(ks[10], (DEPTH, MIX_WIDTH, D_MODEL), MIX_WIDTH ** -0.5),
        'lam_q1': nrm(ks[11], (DEPTH, DA_DK), 0.1),
        'lam_k1': nrm(ks[12], (DEPTH, DA_DK), 0.1),
        'lam_q2': nrm(ks[13], (DEPTH, DA_DK), 0.1),
        'lam_k2': nrm(ks[14], (DEPTH, DA_DK), 0.1),
        'g_subln': 1.0 + nrm(ks[15], (DEPTH, DA_DV), 0.02),
        'rel_bias': nrm(ks[16], (N_BUCKETS, N_DA_HEADS), 0.5),
        'g_sg_ln': 1.0 + nrm(ks[17], (DEPTH, N_SG_GROUPS, SG_CH), 0.02),
        'b_sg_ln': nrm(ks[18], (DEPTH, N_SG_GROUPS, SG_CH), 0.02),
        'w_s': nrm(ks[19], (DEPTH, N_SG_GROUPS, CHUNK, CHUNK), CHUNK ** -0.5),
        'b_s': 1.0 + nrm(ks[20], (DEPTH, N_SG_GROUPS, CHUNK), 0.1),
        'w_rg': nrm(ks[21], (DEPTH, D_MODEL, N_EXPERT_GROUPS), D_MODEL ** -0.5),
        'b_rg': nrm(ks[22], (DEPTH, N_EXPERT_GROUPS), 0.01),
        'w_re': nrm(ks[23], (DEPTH, D_MODEL, N_EXPERTS), D_MODEL ** -0.5),
        'b_re': nrm(ks[24], (DEPTH, N_EXPERTS), 0.01),
        'w_gate': nrm(ks[25], (DEPTH, N_EXPERTS, D_MODEL, D_EXPERT), D_MODEL ** -0.5),
        'w_up': nrm(ks[26], (DEPTH, N_EXPERTS, D_MODEL, D_EXPERT), D_MODEL ** -0.5),
        'w_down': nrm(ks[27], (DEPTH, N_EXPERTS, D_EXPERT, D_MODEL), D_EXPERT ** -0.5),
        'g_final': 1.0 + nrm(ks[28], (D_MODEL,), 0.02),
    }


def reference(x_prompt, x_sample, c_prompt, c_sample, cache_k, cache_v, page_table,
              w_ada, b_ada, w_in, w_o, lam_q1, lam_k1, lam_q2, lam_k2, g_subln, rel_bias,
              g_sg_ln, b_sg_ln, w_s, b_s, w_rg, b_rg, w_re, b_re, w_gate, w_up, w_down, g_final):
    dec_b, n_pages = page_table.shape
    past = n_pages * PAGE_SIZE
    xp, xs = x_prompt, x_sample
    kp_l, vp_l, ks_l, vs_l, sgv_l = [], [], [], [], []
    for l in range(DEPTH):
        p = {'w_ada': w_ada[l], 'b_ada': b_ada[l], 'w_in': w_in[l], 'w_o': w_o[l],
             'g_subln': g_subln[l], 'g_sg_ln': g_sg_ln[l], 'b_sg_ln': b_sg_ln[l],
             'w_s': w_s[l], 'b_s': b_s[l], 'w_rg': w_rg[l], 'b_rg': b_rg[l],
             'w_re': w_re[l], 'b_re': b_re[l], 'w_gate': w_gate[l], 'w_up': w_up[l],
             'w_down': w_down[l]}
        lam_init = 0.8 - 0.6 * math.exp(-0.3 * l)
        lam = (jnp.exp(jnp.sum(lam_q1[l].astype(jnp.float32) * lam_k1[l].astype(jnp.float32)))
               - jnp.exp(jnp.sum(lam_q2[l].astype(jnp.float32) * lam_k2[l].astype(jnp.float32)))
               + lam_init)
        past_k = cache_k[page_table, l].reshape(dec_b, past, N_DA_HEADS, 2, DA_DK)
        past_v = cache_v[page_table, l].reshape(dec_b, past, N_DA_HEADS, DA_DV)

        def att_p(q, k, v, lm):
            return attend_prompt(q, k, v, lm, rel_bias)

        def att_s(q, k, v, lm, pk=past_k, pv=past_v):
            return attend_sample(q, k, v, pk, pv, lm, rel_bias)

        xp, kp, vp, _ = decoder_layer(xp, c_prompt, att_p, p, lam, lam_init)
        xs, ksn, vsn, sgv = decoder_layer(xs, c_sample, att_s, p, lam, lam_init)
        kp_l.append(kp)
        vp_l.append(vp)
        ks_l.append(ksn)
        vs_l.append(vsn)
        sgv_l.append(sgv)
    y_prompt = rmsnorm(xp, g_final)
    y_sample = rmsnorm(xs, g_final)
    new_k_prompt = jnp.stack(kp_l, axis=1)
    new_v_prompt = jnp.stack(vp_l, axis=1)
    new_k_sample = jnp.stack(ks_l, axis=1)
    new_v_sample = jnp.stack(vs_l, axis=1)
    new_sgv_sample = jnp.stack(sgv_l, axis=1)
    return (y_prompt, y_sample, new_k_prompt, new_v_prompt, new_k_sample, new_v_sample, new_sgv_sample)
```

```python
import numpy as np
import concourse.bass as bass
import concourse.mybir as mybir

F32 = mybir.dt.float32
BF16 = mybir.dt.bfloat16
I32 = mybir.dt.int32
AF = mybir.ActivationFunctionType
ALU = mybir.AluOpType
AX = mybir.AxisListType

_ESZ = {}


def _esz(dt):
    s = str(dt)
    if s not in _ESZ:
        if '32' in s:
            _ESZ[s] = 4
        elif '16' in s:
            _ESZ[s] = 2
        elif '64' in s:
            _ESZ[s] = 8
        else:
            _ESZ[s] = 1
    return _ESZ[s]


def box(ap):
    t = ap.tensor
    name = t.name
    esz = _esz(ap.dtype)
    dims = ap.ap
    off = ap.offset
    if not isinstance(off, int):
        return (name, 0, 1 << 30, 0, 1 << 40)
    tn = type(t).__name__
    if tn.startswith('PSum'):
        return ('PSUM:' + name, 0, 128, 0, 1 << 20)
    if tn.startswith('SB'):
        pstep, pcnt = dims[0]
        if pstep == 0:
            pstep = 1 << 40
        p0 = off // pstep + getattr(t, 'base_partition', 0) if pstep < (1 << 40) else 0
        f0 = off % pstep if pstep < (1 << 40) else off
        ext = sum((c - 1) * abs(s) for s, c in dims[1:])
        return (name, p0, p0 + pcnt, f0 * esz, (f0 + ext + 1) * esz)
    ext = sum((c - 1) * abs(s) for s, c in dims)
    return (name, 0, 1, off * esz, (off + ext + 1) * esz)


class Op:
    __slots__ = ('id', 'eng', 'kind', 'fn', 'deps', 'marked', 'rank', 'sem', 'semval', 'waits')

    def __init__(self, id, eng, kind, fn):
        self.id = id
        self.eng = eng
        self.kind = kind
        self.fn = fn
        self.deps = {}
        self.marked = False
        self.rank = 0
        self.sem = None
        self.semval = 0
        self.waits = []


EPOCH = 2000


class Prog:
    def __init__(self, nc, n_dma_sems=40):
        self.nc = nc
        self.ops = []
        self.hist = {}
        self.n_dma_sems = n_dma_sems
        self.track_off = set()

    def _add(self, eng, kind, fn, reads, writes):
        op = Op(len(self.ops), eng, kind, fn)
        self.ops.append(op)
        for ap in reads:
            if ap is None or isinstance(ap, (int, float)):
                continue
            self._access(op, box(ap), False)
        for ap in writes:
            if ap is None:
                continue
            self._access(op, box(ap), True)
        return op

    def _access(self, op, b, is_write):
        name, plo, phi, flo, fhi = b
        if name in self.track_off:
            return
        h = self.hist.setdefault(name, [])
        keep = []
        for e in h:
            eplo, ephi, eflo, efhi, eid, ew = e
            if eid == op.id:
                keep.append(e)
                continue
            ov = not (ephi <= plo or phi <= eplo or efhi <= flo or fhi <= eflo)
            if ov and name.startswith('PSUM:') and not ew and not is_write and self.ops[eid].eng != op.eng:
                op.deps.setdefault(eid, 'rar')
            if ov:
                if ew or is_write:
                    typ = 'raw' if (ew and not is_write) else ('waw' if ew else 'war')
                    old = op.deps.get(eid)
                    if old is None or typ == 'raw':
                        op.deps[eid] = typ
                if is_write and plo <= eplo and ephi <= phi and flo <= eflo and efhi <= fhi:
                    continue
            keep.append(e)
        if not is_write:
            if op.kind == 'c':
                keep2 = []
                for e in keep:
                    if (not e[5]) and e[0] == plo and e[1] == phi and e[2] == flo and e[3] == fhi:
                        o = self.ops[e[4]]
                        if o.kind == 'c' and o.eng == op.eng:
                            continue
                    keep2.append(e)
                keep = keep2
        keep.append((plo, phi, flo, fhi, op.id, is_write))
        self.hist[name] = keep

    def mm(self, out, lhsT, rhs, start=True, stop=True, **kw):
        return self._add('pe', 'c', lambda e: e.matmul(out, lhsT, rhs, start=start, stop=stop, **kw),
                         [lhsT, rhs], [out])

    def tr(self, out, in_, ident):
        return self._add('pe', 'c', lambda e: e.transpose(out, in_, ident), [in_, ident], [out])

    def act(self, out, in_, func, bias=0.0, scale=1.0, accum_out=None):
        rd = [in_]
        if not isinstance(bias, (int, float)):
            rd.append(bias)
        if not isinstance(scale, (int, float)):
            rd.append(scale)
        kw = {}
        if accum_out is not None:
            kw['accum_out'] = accum_out
        return self._add('act', 'c', lambda e: e.activation(out, in_, func, bias=bias, scale=scale, **kw),
                         rd, [out, accum_out])

    def ts(self, eng, out, in0, s1, s2=None, op0=ALU.mult, op1=None, accum_out=None):
        rd = [in0]
        if not isinstance(s1, (int, float)):
            rd.append(s1)
        if s2 is not None and not isinstance(s2, (int, float)):
            rd.append(s2)
        kw = {}
        if op1 is not None:
            kw['op1'] = op1
        if accum_out is not None:
            kw['accum_out'] = accum_out
        return self._add(eng, 'c', lambda e: e.tensor_scalar(out, in0, s1, s2, op0, **kw), rd, [out, accum_out])

    def tt(self, eng, out, in0, in1, op):
        return self._add(eng, 'c', lambda e: e.tensor_tensor(out, in0, in1, op), [in0, in1], [out])

    def stt(self, eng, out, in0, scalar, in1, op0, op1):
        rd = [in0, in1]
        if not isinstance(scalar, (int, float)):
            rd.append(scalar)
        return self._add(eng, 'c', lambda e: e.scalar_tensor_tensor(out, in0, scalar, in1, op0, op1), rd, [out])

    def copy(self, eng, out, in_):
        if eng == 'act':
            return self._add('act', 'c', lambda e: e.copy(out, in_), [in_], [out])
        return self._add(eng, 'c', lambda e: e.tensor_copy(out, in_), [in_], [out])

    def red(self, eng, out, in_, op, axis=AX.X):
        return self._add(eng, 'c', lambda e: e.tensor_reduce(out, in_, axis, op), [in_], [out])

    def recip(self, out, in_):
        return self._add('dve', 'c', lambda e: e.reciprocal(out, in_), [in_], [out])

    def memset(self, eng, ap, val):
        return self._add(eng, 'c', lambda e: e.memset(ap, val), [], [ap])

    def dma(self, q, out, in_, extra_reads=(), **kw):
        kind = 'sw' if q == 'pool' else 'dma'
        return self._add(q, kind, lambda e: e.dma_start(out=out, in_=in_, **kw), [in_] + list(extra_reads), [out])

    def custom(self, eng, kind, fn, reads, writes):
        return self._add(eng, kind, fn, reads, writes)

    def emit(self):
        nc = self.nc
        ops = self.ops
        n_hw = self.n_dma_sems
        n_sw = 8
        last_on_sem = {}
        di = {'dma': 0, 'sw': 0}
        for op in ops:
            if op.kind in ('dma', 'sw'):
                if op.kind == 'dma':
                    s = di['dma'] % n_hw
                else:
                    s = n_hw + di['sw'] % n_sw
                di[op.kind] += 1
                op.sem = s
                prev = last_on_sem.get(s)
                op.semval = (prev.semval if prev else 0) + 16
                if prev is not None:
                    op.deps.setdefault(prev.id, 'waw')
                last_on_sem[s] = op

        def skip(p, op, typ):
            if p.kind != 'c':
                return False
            if p.eng == 'pe' and op.eng == 'pe' and op.kind == 'c':
                return True
            return False

        for op in ops:
            for d, typ in op.deps.items():
                p = ops[d]
                if p.kind == 'c' and not skip(p, op, typ):
                    p.marked = True
        cnt = {}
        for op in ops:
            if op.kind == 'c' and op.marked:
                cnt[op.eng] = cnt.get(op.eng, 0) + 1
                op.rank = cnt[op.eng]
        engs = ['pe', 'act', 'dve', 'pool', 'sp']
        waited = {e: {} for e in engs}
        nwaits = 0
        for op in ops:
            need = {}
            for d, typ in op.deps.items():
                p = ops[d]
                if p.kind == 'c':
                    if skip(p, op, typ):
                        continue
                    key = ('e', p.eng, (p.rank - 1) // EPOCH)
                    val = (p.rank - 1) % EPOCH + 1
                else:
                    key = ('d', p.sem)
                    val = p.semval
                if need.get(key, 0) < val:
                    need[key] = val
            w = waited[op.eng]
            for key, val in need.items():
                if w.get(key, 0) >= val:
                    continue
                w[key] = val
                op.waits.append((key, val))
                nwaits += 1
        self.stats = dict(n_ops=len(ops), n_waits=nwaits, marked=dict(cnt), n_dma=dict(di))
        import contextlib
        with contextlib.ExitStack() as st:
            esem = {}
            for e in ['pe', 'act', 'dve', 'pool']:
                for k in range((cnt.get(e, 0) + EPOCH - 1) // EPOCH + 1):
                    esem[(e, k)] = st.enter_context(nc.semaphore('es_%s_%d' % (e, k)))
            dsem = [st.enter_context(nc.semaphore('ds_%d' % i)) for i in range(n_hw + n_sw)]
            block = st.enter_context(nc.Block())
            per = {e: [o for o in ops if o.eng == e] for e in engs}

            def run(e, eng):
                for op in per[e]:
                    for key, val in op.waits:
                        sem = esem[(key[1], key[2])] if key[0] == 'e' else dsem[key[1]]
                        eng.wait_ge(sem, val)
                    ins = op.fn(eng)
                    if op.kind in ('dma', 'sw'):
                        ins.then_inc(dsem[op.sem], 16)
                    elif op.marked:
                        ins.then_inc(esem[(e, (op.rank - 1) // EPOCH)], 1)
                if e == 'sp':
                    for s_, o in last_on_sem.items():
                        eng.wait_ge(dsem[s_], o.semval)

            @block.tensor
            def _(eng):
                run('pe', eng)

            @block.scalar
            def _(eng):
                run('act', eng)

            @block.vector
            def _(eng):
                run('dve', eng)

            @block.gpsimd
            def _(eng):
                run('pool', eng)

            @block.sync
            def _(eng):
                run('sp', eng)

import contextlib
import ml_dtypes
from concourse.bass_utils import run_bass_kernel_spmd

D = 1024
EPS = 1e-6
LAM_INIT = 0.2
NCORES = 8
NEG = -30000.0


def _own_block(cpar, t):
    return 2 * t + ((t + cpar) % 2)


def _partner_block(cpar, t):
    return 2 * t + 1 - ((t + cpar) % 2)


def _pblk(t, pos):
    return (t // 2) * 4 + (t % 2) + 2 * pos


import os
STAGE = int(os.environ.get('MK_STAGE', '9'))
SUB = int(os.environ.get('MK_SUB', '99'))


def _chk(n):
    if SUB <= n:
        raise _Stop()


class _Stop(Exception):
    pass


def build_program(do_sample=True):
    nc = bass.Bass("TRN2", target_bir_lowering=False)

    def din(name, shape, dt=F32):
        return nc.dram_tensor(name, list(shape), dt, kind="ExternalInput").ap()

    def dout(name, shape, dt=F32):
        return nc.dram_tensor(name, list(shape), dt, kind="ExternalOutput").ap()

    xp = din("xp", [4096, D])
    xs = din("xs", [32, D])
    cin = din("cin", [5, D])
    w_ada = din("w_ada", [D, 6 * D])
    b_ada = din("b_ada", [1, 6 * D])
    w_in = din("w_in", [D, 2560])
    w_o = din("w_o", [D, D])
    lam4 = din("lam4", [1, 256])
    gsub_in = din("gsub", [128, 1])
    biasg = din("biasg", [128, 5 * 4 * 128])
    maskg = din("maskg", [128, 5 * 128])
    b31_in = din("b31", [1, 4])
    gsg_in = din("gsg", [1, 512])
    bsg_in = din("bsg", [1, 512])
    wsT_in = din("wsT", [128, 4 * 128])
    tril_in = din("tril", [128, 128])
    wsS_in = din("wsS", [32, 4 * 32])
    trilS_in = din("trilS", [32, 32])
    bs_in = din("bs", [1, 512])
    bsS_in = din("bsS", [1, 128])
    wr_in = din("wr", [D, 20])
    br_in = din("br", [1, 20])
    w_gate = din("w_gate", [16, D, 512])
    w_up = din("w_up", [16, D, 512])
    w_down = din("w_down", [16, 512, D])
    gfin_in = din("gfin", [1, D])
    ident_in = din("ident", [128, 128], BF16)
    identf_in = din("identf", [128, 128])
    sel_in = din("sel", [5, 160])
    if do_sample:
        cache_k = din("cache_k", [2560 * 128, 512])
        cache_v = din("cache_v", [2560 * 128, 512])
        pt_in = din("pt", [1, 256], I32)
        sbias_in = din("sbias", [128, 136])
        smask_in = din("smask", [128, 136])
        sb31_in = din("sb31", [128, 1])
        dsel_in = din("dsel", [128, 64])
        pidx_in = din("pidx", [128, 1])

    yp = dout("yp", [2048, D])
    ys = dout("ys", [32, D])
    kp = dout("kp", [2048, 512])
    vp = dout("vp", [2048, 512])
    ks = dout("ks", [32, 512])
    vso = dout("vso", [32, 512])
    sgv = dout("sgv", [32, 512])
    x1scr = nc.dram_tensor("x1scr", [2176, D], F32, kind="Internal").ap()

    P = Prog(nc)
    with contextlib.ExitStack() as st:
      try:
            def sb(name, shape, dt):
                return st.enter_context(nc.sbuf_tensor("s_" + name, list(shape), dt))

            A1 = sb("A1", [128, 52224], BF16)
            A2 = sb("A2", [128, 20480], BF16)
            idb = sb("idb", [128, 128], BF16)
            idf = sb("idf", [128, 128], F32)
            onesb = sb("onesb", [128, 128], BF16)
            modT = sb("modT", [128, 48, 5], F32)
            G1p = sb("G1p", [128, D], BF16)
            G2p = sb("G2p", [128, D], BF16)
            G1s = sb("G1s", [32, D], BF16)
            G2s = sb("G2s", [32, D], BF16)
            EB = sb("EB", [128, 5, 4, 128], BF16)
            bsSB = sb("bsSB", [128, 128], F32)
            wts = sb("wts", [128, 4, 128], BF16)
            wtsS = sb("wtsS", [32, 4, 32], BF16)
            wrt = sb("wrt", [128, 8, 20], F32)
            brB = sb("brB", [128, 20], F32)
            sml = sb("sml", [128, 64], F32)
            ssb = sb("ssb", [128, 64], F32)
            xt = [sb("xt%d" % i, [128, D], F32) for i in range(2)]
            sq = sb("sq", [128, D], BF16)
            xn = [sb("xn%d" % i, [128, D], BF16) for i in range(4)]
            W3 = sb("W3", [128, 13312], BF16)

            QT = A1[:, 0:8192].rearrange("p (h t) -> p h t", h=4)
            SGT = A1[:, 8192:16384].rearrange("p (g t) -> p g t", g=4)
            KT = A1[:, 16384:32768].rearrange("p (h t) -> p h t", h=4)
            VS = A1[:, 32768:49152].rearrange("p (b c) -> p b c", b=32)
            TAIL = A1[:, 49152:52224].bitcast(F32)
            gsgB = TAIL[:, 0:512]
            bsgB = TAIL[:, 512:1024]
            bsB = TAIL[:, 1024:1536]
            H2T = A1[:, 16384:16384 + 8 * 2176].rearrange("p (k t) -> p k t", k=8)
            yaccA = A1[:, 0:16384].bitcast(F32).rearrange("p (b d) -> p b d", d=D)
            yaccB = A1[:, 33792:33792 + 18432].bitcast(F32).rearrange("p (b d) -> p b d", d=D)

            def yacc(blk):
                return yaccA[:, blk, :] if blk < 8 else yaccB[:, blk - 8, :]

            WIN = A2[:, 0:20480].rearrange("p (k n) -> p k n", k=8)
            WO = A2[:, 0:8192].rearrange("p (k n) -> p k n", k=8)
            ESLOT = [A2[:, i * 4096:(i + 1) * 4096] for i in range(4)]
            RTALL = A2[:, 16384:16384 + 2048].bitcast(F32)
            LG = RTALL[:, 0:340].rearrange("p (b n) -> p b n", b=17)
            COMB = RTALL[:, 340:612].rearrange("p (b n) -> p b n", b=17)
            RT = RTALL[:, 612:1020].rearrange("p (b n) -> p b n", b=17)
            hT = W3[:, 0:4096].rearrange("p (k t) -> p k t", k=8)
            kst = W3[:, 4096:5120].bitcast(F32)
            vst = W3[:, 5120:6144].bitcast(F32)
            k16 = W3[:, 6144:6656]
            uT = W3[:, 6656:7680].rearrange("p (g t) -> p g t", g=4)
            vln = W3[:, 7680:8704].bitcast(F32)
            vlb = W3[:, 8704:9216]
            sqv = W3[:, 9216:10240].bitcast(F32)
            gtmp = W3[:, 10240:11264].bitcast(F32)
            wach = [W3[:, 4096 + i * 2048: 4096 + (i + 1) * 2048].rearrange("p (k n) -> p k n", k=8) for i in range(2)]
            mrow = W3[:, 0:512].bitcast(F32)
            bac = W3[:, 512:1024].bitcast(F32)
            PT = [W3[:, 11264 + i * 512: 11264 + (i + 1) * 512] for i in range(4)]
            ep_rl = W3[:, 0:1024].bitcast(F32)
            ep_on0 = W3[:, 1024:2048].bitcast(F32)
            ep_on1 = W3[:, 2048:3072].bitcast(F32)
            ep_o = W3[:, 3072:4096].bitcast(F32)
            ep_sq = W3[:, 4096:4608]
            ep_r = W3[:, 4608:5632].bitcast(F32)
            h2Tf = W3[:, 0:2048].bitcast(F32).rearrange("p (k t) -> p k t", k=8)
            x1b = W3[:, 2048:4096].bitcast(F32)
            xnf = W3[:, 4096:6144].bitcast(F32)
            tmpf = W3[:, 6144:8192].bitcast(F32)
            he = [W3[:, 8192 + i * 2048: 8192 + (i + 1) * 2048].rearrange("p (f t) -> p f t", f=4) for i in range(2)]
            sil = W3[:, 12288:13312].bitcast(F32)

            pb = [st.enter_context(nc.psum_tensor("pb%d" % i, [128, 512], F32)) for i in range(8)]
            pbi = [0]

            nbanks = [8]

            def nextbank():
                b = pb[pbi[0] % nbanks[0]]
                pbi[0] += 1
                return b

            ssi = [0]

            def sscols(n=4):
                i = ssi[0] % 16
                ssi[0] += 1
                return ssb[:, i * 4:(i * 4 + n)]

            evi = [0]

            def evac_eng():
                evi[0] += 1
                return 'dve' if evi[0] % 2 else 'act'

            def affine(eng, out, in_, s_ap, b_ap):
                if eng == 'act':
                    P.act(out, in_, AF.Identity, bias=b_ap, scale=s_ap)
                else:
                    P.ts(eng, out, in_, s_ap, b_ap, op0=ALU.mult, op1=ALU.add)

            def pcopy(eng, out, in_):
                if eng == 'act':
                    P.act(out, in_, AF.Copy)
                else:
                    P.copy(eng, out, in_)

            P.dma('sp', idb[:], ident_in)
            P.dma('sp', idf[:], identf_in)
            P.memset('dve', onesb[:], 1.0)
            P.dma('sp', gsgB, gsg_in.to_broadcast([128, 512]))
            P.dma('sp', bsgB, bsg_in.to_broadcast([128, 512]))
            P.dma('sp', bsB, bs_in.to_broadcast([128, 512]))
            P.dma('sp', bsSB[:], bsS_in.to_broadcast([128, 128]))
            P.dma('sp', brB[:], br_in.to_broadcast([128, 20]))
            P.dma('sp', wrt[:], wr_in.rearrange("(k p) n -> p k n", p=128))
            P.dma('sp', sml[:, 8:9], gsub_in)
            P.dma('sp', sml[:, 12:16], b31_in.to_broadcast([128, 4]))
            lamt = gtmp[:, 0:256]
            P.dma('sp', lamt, lam4.to_broadcast([128, 256]))
            P.tt('dve', sqv[:, 0:64], lamt[:, 0:64], lamt[:, 64:128], ALU.mult)
            P.tt('dve', sqv[:, 64:128], lamt[:, 128:192], lamt[:, 192:256], ALU.mult)
            P.red('dve', sml[:, 0:2], sqv[:, 0:128].rearrange("p (a b) -> p a b", a=2), ALU.add)
            P.act(sml[:, 2:4], sml[:, 0:2], AF.Exp)
            P.tt('dve', sml[:, 4:5], sml[:, 3:4], sml[:, 2:3], ALU.subtract)
            P.ts('dve', sml[:, 5:6], sml[:, 4:5], -LAM_INIT, None, op0=ALU.add)
            neglam = sml[:, 5:6]
            P.ts('dve', sml[:, 9:10], sml[:, 8:9], 1.0 - LAM_INIT, None, op0=ALU.mult)
            gsubc = sml[:, 9:10]
            P.dma('sp', sqv[:, 0:512], wsT_in)
            P.dma('sp', gtmp[:, 0:128], tril_in)
            for g in range(4):
                P.tt('dve', wts[:, g, :], sqv[:, g * 128:(g + 1) * 128], gtmp[:, 0:128], ALU.mult)
            P.dma('sp', vln[0:32, 0:128], wsS_in)
            P.dma('sp', vln[0:32, 128:160], trilS_in)
            for g in range(4):
                P.tt('dve', wtsS[:, g, :], vln[0:32, g * 32:(g + 1) * 32], vln[0:32, 128:160], ALU.mult)

            cint = xt[0][0:5, :]
            P.dma('sp', cint, cin)
            scs = xt[1][0:5, :]
            P.act(scs, cint, AF.Silu)
            b0 = nextbank()
            for kc in range(8):
                P.tr(b0[:, kc * 5:(kc + 1) * 5], scs[:, kc * 128:(kc + 1) * 128], idf[0:5, 0:5])
            scT = sq[:, 0:40].rearrange("p (k r) -> p k r", k=8)
            P.copy('dve', scT, b0[:, 0:40].rearrange("p (k r) -> p k r", k=8))
            selt = sml[0:5, 16:16 + 0]
            selsb = sb("selsb", [5, 160], F32)
            P.dma('sp', selsb[:], sel_in)
            for n in range(24):
                wa = wach[n % 2]
                P.dma('pool', wa, w_ada[:, n * 256:(n + 1) * 256].rearrange("(k p) n -> p k n", p=128))
                P.dma('sp', bac[0:5, 0:256], b_ada[:, n * 256:(n + 1) * 256].to_broadcast([5, 256]))
                bk = nextbank()
                for kc in range(8):
                    P.mm(bk[0:5, 0:256], scT[:, kc, :], wa[:, kc, :], start=(kc == 0), stop=(kc == 7))
                P.tt('dve', mrow[0:5, 0:256], bk[0:5, 0:256], bac[0:5, 0:256], ALU.add)
                bk2 = nextbank()
                for j in range(2):
                    P.tr(bk2[:, j * 5:(j + 1) * 5], mrow[0:5, j * 128:(j + 1) * 128], idf[0:5, 0:5])
                P.copy('dve', modT[:, n * 2:(n + 1) * 2, :], bk2[:, 0:10].rearrange("p (k r) -> p k r", k=2))
                part = n // 4
                if part in (2, 5):
                    Gp, Gs = (G1p, G1s) if part == 2 else (G2p, G2s)
                    c0 = (n % 4) * 256
                    bk3 = nextbank()
                    P.mm(bk3[:, 0:256], selsb[0:5, 0:128], mrow[0:5, 0:256])
                    P.copy('dve', Gp[:, c0:c0 + 256], bk3[:, 0:256])
                    bk4 = nextbank()
                    P.mm(bk4[0:32, 0:256], selsb[0:5, 128:160], mrow[0:5, 0:256])
                    P.copy('dve', Gs[:, c0:c0 + 256], bk4[0:32, 0:256])
            for ch0 in (8, 32):
                P.ts('dve', modT[:, ch0:ch0 + 8, :], modT[:, ch0:ch0 + 8, :], 1.0, None, op0=ALU.add)

            ebsrc = [xt[0][:, :], xt[1][:, :]]
            P.dma('sp', xt[0][:, 0:1024], biasg[:, 0:1024])
            P.dma('sp', xt[1][:, 0:1024], biasg[:, 1024:2048])
            P.dma('sp', xnf[:, 0:512], biasg[:, 2048:2560])
            P.dma('sp', tmpf[:, 0:640], maskg)
            P.ts('dve', sml[:, 16:20], sml[:, 12:16], -1.0, None, op0=ALU.mult)
            for i in range(5):
                for h in range(4):
                    col = (i * 4 + h) * 128
                    src = (xt[0][:, col:col + 128] if col < 1024 else
                           xt[1][:, col - 1024:col - 1024 + 128] if col < 2048 else xnf[:, col - 2048:col - 2048 + 128])
                    P.act(x1b[:, 0:128], src, AF.Exp, bias=sml[:, 16 + h:17 + h], scale=1.0)
                    P.tt('dve', EB[:, i, h, :], x1b[:, 0:128], tmpf[:, i * 128:(i + 1) * 128], ALU.mult)

            if STAGE <= 1:
                raise _Stop()
            for k in range(8):
                P.dma('pool', WIN[:, k, :], w_in[k * 128:(k + 1) * 128, :])

            def norm_rows(x_ap, rows, out_ap):
                s = sscols(4)
                P.act(sq[0:rows, :], x_ap, AF.Square, accum_out=s[0:rows, 0:1])
                P.act(s[0:rows, 1:2], s[0:rows, 0:1], AF.Sqrt, bias=EPS, scale=1.0 / D)
                P.recip(s[0:rows, 2:3], s[0:rows, 1:2])
                P.ts('dve', out_ap, x_ap, s[0:rows, 2:3], None, op0=ALU.mult)

            def ln_groups(ps, rows, out_f32, out_bf):
                s = sscols(4)
                s2 = sscols(4)
                s3 = sscols(4)
                s4 = sscols(4)
                pv = ps.rearrange("p (g c) -> p g c", g=4)
                P.red('dve', s[0:rows, :], pv, ALU.add)
                P.act(sqv[0:rows, :], ps, AF.Square)
                P.red('dve', s2[0:rows, :], sqv[0:rows, :].rearrange("p (g c) -> p g c", g=4), ALU.add)
                P.ts('dve', s[0:rows, :], s[0:rows, :], 1.0 / 128, None, op0=ALU.mult)
                P.tt('dve', s3[0:rows, :], s[0:rows, :], s[0:rows, :], ALU.mult)
                P.stt('dve', s2[0:rows, :], s2[0:rows, :], 1.0 / 128, s3[0:rows, :], ALU.mult, ALU.subtract)
                P.act(s3[0:rows, :], s2[0:rows, :], AF.Sqrt, bias=EPS, scale=1.0)
                P.recip(s4[0:rows, :], s3[0:rows, :])
                P.stt('dve', s3[0:rows, :], s[0:rows, :], -1.0, s4[0:rows, :], ALU.mult, ALU.mult)
                for g in range(4):
                    P.ts('dve', gtmp[0:rows, g * 128:(g + 1) * 128], ps[:, g * 128:(g + 1) * 128],
                         s4[0:rows, g:g + 1], s3[0:rows, g:g + 1], op0=ALU.mult, op1=ALU.add)
                P.tt('dve', gtmp[0:rows, :], gtmp[0:rows, :], gsgB[0:rows, :], ALU.mult)
                P.tt('dve', out_f32, gtmp[0:rows, :], bsgB[0:rows, :], ALU.add)
                P.copy('dve', out_bf, out_f32)

            for g in range(int(os.environ.get('MK_NG', '8'))):
                for j in range(4):
                    blk = g * 4 + j
                    xb = xt[blk % 2]
                    P.dma('sp', xb[:], xp[blk * 128:(blk + 1) * 128, :])
                    norm_rows(xb[:], 128, xn[j][:])
                _chk(1)
                for dkp in range(4):
                    bk = nextbank()
                    pT = bk[:].bitcast(BF16)
                    for d2 in range(2):
                        dk = dkp * 2 + d2
                        for j in range(4):
                            P.tr(pT[:, d2 * 512 + j * 128: d2 * 512 + (j + 1) * 128], xn[j][:, dk * 128:(dk + 1) * 128], idb[:])
                    for d2 in range(2):
                        dk = dkp * 2 + d2
                        affine(evac_eng(), hT[:, dk, :], pT[:, d2 * 512:(d2 + 1) * 512], modT[:, 8 + dk, 0:1], modT[:, dk, 0:1])
                _chk(2)
                for j in range(4):
                    pblk = g * 4 + j
                    own = j < 2
                    orow = (2 * g + j) * 128
                    bk = nextbank()
                    for dk in range(8):
                        P.mm(bk[:], hT[:, dk, j * 128:(j + 1) * 128], WIN[:, dk, 512:1024], start=(dk == 0), stop=(dk == 7))
                    if own:
                        P.act(kst, bk[:], AF.Copy)
                        P.dma('sp', kp[orow:orow + 128, :], kst)
                    P.copy('dve', k16, bk[:])
                    bk2 = nextbank()
                    pKT = bk2[:].bitcast(BF16)
                    for h in range(4):
                        P.tr(pKT[:, h * 128:(h + 1) * 128], k16[:, h * 128:(h + 1) * 128], idb[:])
                    pcopy(evac_eng(), KT[:, :, pblk * 128:(pblk + 1) * 128], pKT[:, 0:512].rearrange("p (h t) -> p h t", h=4))
                    bk = nextbank()
                    for dk in range(8):
                        P.mm(bk[:], hT[:, dk, j * 128:(j + 1) * 128], WIN[:, dk, 1024:1536], start=(dk == 0), stop=(dk == 7))
                    if own:
                        P.act(vst, bk[:], AF.Copy)
                        P.dma('sp', vp[orow:orow + 128, :], vst)
                    P.copy('dve', VS[:, pblk, :], bk[:])
                _chk(3)
                t0 = 2 * g * 128
                for hh in range(2):
                    bk = nextbank()
                    for h2 in range(2):
                        h = hh * 2 + h2
                        for dk in range(8):
                            P.mm(bk[:, h2 * 256:(h2 + 1) * 256], WIN[:, dk, h * 128:(h + 1) * 128], hT[:, dk, 0:256],
                                 start=(dk == 0), stop=(dk == 7))
                    pcopy(evac_eng(), QT[:, hh * 2:hh * 2 + 2, t0:t0 + 256], bk[:].rearrange("p (h t) -> p h t", h=2))
                for hh in range(2):
                    bk = nextbank()
                    for h2 in range(2):
                        gi = hh * 2 + h2
                        for dk in range(8):
                            P.mm(bk[:, h2 * 256:(h2 + 1) * 256], WIN[:, dk, 1536 + gi * 128:1536 + (gi + 1) * 128], hT[:, dk, 0:256],
                                 start=(dk == 0), stop=(dk == 7))
                    pcopy(evac_eng(), uT[:, hh * 2:hh * 2 + 2, :], bk[:].rearrange("p (h t) -> p h t", h=2))
                for j in range(2):
                    bk = nextbank()
                    for dk in range(8):
                        P.mm(bk[:], hT[:, dk, j * 128:(j + 1) * 128], WIN[:, dk, 2048:2560], start=(dk == 0), stop=(dk == 7))
                    ln_groups(bk[:], 128, vln, vlb)
                    bk2 = nextbank()
                    for gi in range(4):
                        P.mm(bk2[:, gi * 128:(gi + 1) * 128], vlb[:, gi * 128:(gi + 1) * 128], wts[:, gi, :])
                    P.tt('dve', gtmp, bk2[:], bsB, ALU.add)
                    tt0 = t0 + j * 128
                    P.tt('dve', SGT[:, :, tt0:tt0 + 128], gtmp.rearrange("p (g t) -> p g t", g=4),
                         uT[:, :, j * 128:(j + 1) * 128], ALU.mult)

            if os.environ.get('MK_NOS'):
                raise _Stop()
            hTs = hT[:, :, 0:32]
            xsb = xt[0][0:32, :]
            P.dma('sp', xsb, xs)
            norm_rows(xsb, 32, xn[0][0:32, :])
            bk = nextbank()
            pT = bk[:].bitcast(BF16)
            for dk in range(8):
                P.tr(pT[:, dk * 32:(dk + 1) * 32], xn[0][0:32, dk * 128:(dk + 1) * 128], idb[0:32, 0:32])
            for dk in range(8):
                for s in range(4):
                    affine('dve', hT[:, dk, s * 8:(s + 1) * 8], pT[:, dk * 32 + s * 8: dk * 32 + (s + 1) * 8],
                           modT[:, 8 + dk, 1 + s:2 + s], modT[:, dk, 1 + s:2 + s])
            SK = sb("SK", [128, 4, 32], BF16)
            SVs = [xn[1 + s_ // 2][0:8, (s_ % 2) * 512:(s_ % 2 + 1) * 512] for s_ in range(4)]
            SQ = sb("SQ", [128, 4, 32], BF16)
            SGS = sb("SGS", [128, 4, 32], BF16)
            bk = nextbank()
            for dk in range(8):
                P.mm(bk[0:32, :], hT[:, dk, 0:32], WIN[:, dk, 512:1024], start=(dk == 0), stop=(dk == 7))
            P.act(kst[0:32, :], bk[0:32, :], AF.Copy)
            P.dma('sp', ks, kst[0:32, :])
            P.copy('dve', k16[0:32, :], bk[0:32, :])
            bk2 = nextbank()
            pKT = bk2[:].bitcast(BF16)
            for h in range(4):
                P.tr(pKT[:, h * 32:(h + 1) * 32], k16[0:32, h * 128:(h + 1) * 128], idb[0:32, 0:32])
            P.copy('dve', SK[:], pKT[:, 0:128].rearrange("p (h t) -> p h t", h=4))
            bk = nextbank()
            for dk in range(8):
                P.mm(bk[0:32, :], hT[:, dk, 0:32], WIN[:, dk, 1024:1536], start=(dk == 0), stop=(dk == 7))
            P.act(vst[0:32, :], bk[0:32, :], AF.Copy)
            P.dma('sp', vso, vst[0:32, :])
            for s_ in range(4):
                bk = nextbank()
                for dk in range(8):
                    P.mm(bk[0:8, :], hT[:, dk, s_ * 8:(s_ + 1) * 8], WIN[:, dk, 1024:1536], start=(dk == 0), stop=(dk == 7))
                P.copy('dve', SVs[s_], bk[0:8, :])
            bk = nextbank()
            for h in range(4):
                for dk in range(8):
                    P.mm(bk[:, h * 32:(h + 1) * 32], WIN[:, dk, h * 128:(h + 1) * 128], hT[:, dk, 0:32], start=(dk == 0), stop=(dk == 7))
            P.copy('dve', SQ[:], bk[:, 0:128].rearrange("p (h t) -> p h t", h=4))
            bk = nextbank()
            for gi in range(4):
                for dk in range(8):
                    P.mm(bk[:, gi * 32:(gi + 1) * 32], WIN[:, dk, 1536 + gi * 128:1536 + (gi + 1) * 128], hT[:, dk, 0:32],
                         start=(dk == 0), stop=(dk == 7))
            P.copy('dve', uT[:, :, 0:32], bk[:, 0:128].rearrange("p (h t) -> p h t", h=4))
            bk = nextbank()
            for dk in range(8):
                P.mm(bk[0:32, :], hT[:, dk, 0:32], WIN[:, dk, 2048:2560], start=(dk == 0), stop=(dk == 7))
            ln_groups(bk[0:32, :], 32, vln[0:32, :], vlb[0:32, :])
            P.dma('sp', sgv, vln[0:32, :])
            bk2 = nextbank()
            for gi in range(4):
                P.mm(bk2[:, gi * 32:(gi + 1) * 32], vlb[0:32, gi * 128:(gi + 1) * 128], wtsS[0:32, gi, :])
            P.tt('dve', gtmp[:, 0:128], bk2[:, 0:128], bsSB[:], ALU.add)
            P.tt('dve', SGS[:], gtmp[:, 0:128].rearrange("p (g t) -> p g t", g=4), uT[:, :, 0:32], ALU.mult)

            if STAGE <= 2:
                raise _Stop()
            P.memset('dve', sml[:, 20:22], 0.0)
            for (src, nch, col) in ((QT, 4, 20), (KT, 8, 21)):
                for h in range(4):
                    for c in range(nch):
                        P.act(PT[0], src[:, h, c * 512:(c + 1) * 512], AF.Square)
                        bk = nextbank()
                        P.mm(bk[:], onesb[:], PT[0])
                        s = sscols(4)
                        P.red('dve', s[:, 0:1], bk[:], ALU.max)
                        P.tt('dve', sml[:, col:col + 1], sml[:, col:col + 1], s[:, 0:1], ALU.max)
            P.tt('dve', sml[:, 22:23], sml[:, 20:21], sml[:, 21:22], ALU.add)
            P.ts('dve', sml[:, 23:24], sml[:, 22:23], -1.02 / 16.0, None, op0=ALU.mult)
            negC = sml[:, 23:24]

            PS = [pb[0], pb[1], pb[2], pb[3]]
            PO = [pb[4], pb[5]]
            PL = [pb[6], pb[7]]
            it = [0]
            for c in range(4):
                for h in range(4):
                    kbs = []
                    for u in range(4 * c + 4):
                        for pos in range(2):
                            r = max(u - 4 * c, 0)
                            sp_list = []
                            if pos == 0:
                                if u >= 4 * c:
                                    sp_list.append(((u - 4 * c) * 128, 0))
                                if u + 1 >= 4 * c and u + 1 < 4 * c + 4:
                                    sp_list.append(((u + 1 - 4 * c) * 128, 3 + (u + 1) % 2))
                            else:
                                if u >= 4 * c:
                                    sp_list.append(((u - 4 * c) * 128, 1 + u % 2))
                            kbs.append((_pblk(u, pos), r * 128, sp_list))
                    for ki, (pblk, col0, sp_list) in enumerate(kbs):
                        first = ki == 0
                        last = ki == len(kbs) - 1
                        for m in range(2):
                            ps_ = PS[(it[0] % 2) * 2 + m]
                            pt_ = PT[(it[0] % 2) * 2 + m]
                            P.mm(ps_[:, col0:512], KT[m * 64:(m + 1) * 64, h, pblk * 128:(pblk + 1) * 128],
                                 QT[m * 64:(m + 1) * 64, h, c * 512 + col0:(c + 1) * 512])
                            P.act(pt_[:, col0:512], ps_[:, col0:512], AF.Exp, bias=negC, scale=0.125)
                            for (a, idx) in sp_list:
                                P.tt('dve', pt_[:, a:a + 128], pt_[:, a:a + 128], EB[:, idx, h, :], ALU.mult)
                        for m in range(2):
                            pt_ = PT[(it[0] % 2) * 2 + m]
                            P.mm(PO[m][:, col0:512], VS[:, pblk, h * 128:(h + 1) * 128], pt_[:, col0:512], start=first, stop=last)
                            P.mm(PL[m][:, col0:512], onesb[:], pt_[:, col0:512], start=first, stop=last)
                        it[0] += 1
                    for m, on in ((0, ep_on0), (1, ep_on1)):
                        P.recip(ep_rl, PL[m][:])
                        P.tt('dve', on, PO[m][:], ep_rl, ALU.mult)
                    P.stt('dve', ep_o, ep_on1, neglam, ep_on0, ALU.mult, ALU.add)
                    P.act(ep_sq, ep_o, AF.Square)
                    bx = PS[0]
                    P.mm(bx[:], onesb[:], ep_sq)
                    P.act(ep_r, bx[:], AF.Sqrt, bias=EPS, scale=1.0 / 128)
                    P.recip(ep_rl, ep_r)
                    P.stt('dve', QT[:, h, c * 512:(c + 1) * 512], ep_o, gsubc, ep_rl, ALU.mult, ALU.mult)
            AOT = QT

            if STAGE <= 3:
                raise _Stop()
            for k in range(8):
                P.dma('pool', WO[:, k, :], w_o[k * 128:(k + 1) * 128, :])

            def b2_block(blk, rows, mT_chunks, x_src_ap, Gt, G2unused=None):
                xb = xt[blk % 2]
                P.dma('sp', xb[0:rows, :], x_src_ap)
                for half in range(2):
                    bk = nextbank()
                    for fc in range(8):
                        P.mm(bk[0:rows, :], mT_chunks[fc], WO[:, fc, half * 512:(half + 1) * 512], start=(fc == 0), stop=(fc == 7))
                    P.tt('dve', tmpf[0:rows, half * 512:(half + 1) * 512], bk[0:rows, :], Gt[0:rows, half * 512:(half + 1) * 512], ALU.mult)
                P.tt('dve', x1b[0:rows, :], tmpf[0:rows, :], xb[0:rows, :], ALU.add)
                P.dma('sp', x1scr[blk * 128:blk * 128 + rows, :], x1b[0:rows, :])
                norm_rows(x1b[0:rows, :], rows, xnf[0:rows, :])
                for hf in range(2):
                    bk = nextbank()
                    for d4 in range(4):
                        dk = hf * 4 + d4
                        P.tr(bk[:, d4 * 128:d4 * 128 + rows], xnf[0:rows, dk * 128:(dk + 1) * 128], idf[0:rows, 0:rows])
                    for d4 in range(4):
                        dk = hf * 4 + d4
                        if rows == 128:
                            affine(evac_eng(), h2Tf[:, dk, :], bk[:, d4 * 128:(d4 + 1) * 128], modT[:, 32 + dk, 0:1], modT[:, 24 + dk, 0:1])
                        else:
                            for s in range(4):
                                affine('dve', h2Tf[:, dk, s * 8:(s + 1) * 8], bk[:, d4 * 128 + s * 8:d4 * 128 + (s + 1) * 8],
                                       modT[:, 32 + dk, 1 + s:2 + s], modT[:, 24 + dk, 1 + s:2 + s])
                P.copy('dve', H2T[:, :, blk * 128:blk * 128 + rows], h2Tf[:, :, 0:rows])
                bk = nextbank()
                for dk in range(8):
                    P.mm(bk[0:rows, 0:20], h2Tf[:, dk, 0:rows], wrt[:, dk, :], start=(dk == 0), stop=(dk == 7))
                P.tt('dve', LG[0:rows, blk, :], bk[0:rows, 0:20], brB[0:rows, :], ALU.add)

            for t in range(16):
                chunks = [AOT[:, h, t * 128:(t + 1) * 128] for h in range(4)] + [SGT[:, g, t * 128:(t + 1) * 128] for g in range(4)]
                prow = _pblk(t, 0) * 128
                b2_block(t, 128, chunks, xp[prow:prow + 128, :], G1p)

            if STAGE <= 4:
                raise _Stop()
            SAO = sb("SAO", [128, 4, 32], BF16)
            if do_sample:

                S_all = A1[:, 33792:33792 + 16400].bitcast(F32)
                Pm = A1[:, 0:8200]
                ktl = [A1[:, 8704 + i * 1024: 8704 + (i + 1) * 1024].bitcast(F32) for i in range(3)]
                vtl = [A1[:, 11776 + i * 1024: 11776 + (i + 1) * 1024].bitcast(F32) for i in range(3)]
                vbl = [A1[:, 14848 + i * 512: 14848 + (i + 1) * 512] for i in range(2)]
                KTp = [A2[:, 8192 + i * 512: 8192 + (i + 1) * 512].rearrange("p (h t) -> p h t", h=4) for i in range(2)]
                PTp = [A2[:, 9216 + i * 128: 9216 + (i + 1) * 128] for i in range(2)]
                idx = A2[:, 9472:9984].bitcast(I32)
                ptb = A2[:, 9984:10496].bitcast(I32)
                SBT = A2[:, 10496:10768].bitcast(F32)
                smk = A2[:, 10768:11040].bitcast(F32)
                dsl = A2[:, 11040:11168].bitcast(F32)
                Dm = A2[:, 11168:11232].bitcast(F32)
                On = A2[:, 11232:11488].bitcast(F32)
                osb = A2[:, 11488:11744].bitcast(F32)
                PTn = A2[:, 11744:11872]
                sm2 = A2[:, 11872:11936].bitcast(F32)
                QB = xn[3][:, 0:512].rearrange("p (s h c) -> p s h c", s=4, h=4)
                P.dma('sp', ptb, pt_in.to_broadcast([128, 256]))
                P.dma('sp', sm2[:, 0:1], pidx_in)
                P.dma('sp', sm2[:, 1:2], sb31_in)
                P.dma('sp', SBT, sbias_in)
                P.dma('sp', smk, smask_in)
                P.dma('sp', dsl, dsel_in)
                P.ts('dve', idx, ptb, 128.0, sm2[:, 0:1], op0=ALU.mult, op1=ALU.add)
                P.ts('dve', SBT, SBT, sm2[:, 1:2], None, op0=ALU.subtract)
                P.tt('dve', SBT, SBT, smk, ALU.add)
                P.stt('dve', Dm, dsl[:, 32:64], neglam, dsl[:, 0:32], ALU.mult, ALU.add)
                P.memset('dve', QB, 0.0)
                for h in range(4):
                    P.copy('dve', QB[0:64, :, h, 0:8], SQ[0:64, h, :].rearrange("p (s q) -> p s q", s=4))
                    P.copy('dve', QB[64:128, :, h, 8:16], SQ[64:128, h, :].rearrange("p (s q) -> p s q", s=4))
                nbanks[0] = 7
                bO = pb[7]

                def gather(dst, table, col):
                    off = bass.IndirectOffsetOnAxis(ap=idx[:, col:col + 1], axis=0)
                    P.custom('pool', 'sw',
                             lambda e: e.indirect_dma_start(out=dst, out_offset=None, in_=table, in_offset=off),
                             [idx[:, col:col + 1]], [dst])

                def tp(h):
                    return dict(tile_position=(0, 96)) if h == 3 else {}

                gi = [0]
                for s_ in range(4):
                    bS = None
                    for j in range(64):
                        kt = ktl[gi[0] % 3]
                        ktp = KTp[gi[0] % 2]
                        gi[0] += 1
                        gather(kt, cache_k, s_ * 64 + j)
                        bkT = nextbank()
                        for h in range(4):
                            P.tr(bkT[:, h * 128:(h + 1) * 128], kt[:, h * 128:(h + 1) * 128], idf[:])
                        pcopy(evac_eng(), ktp, bkT[:].rearrange("p (h t) -> p h t", h=4))
                        if j % 4 == 0:
                            bS = nextbank()
                        for h in range(4):
                            P.mm(bS[32 * h:32 * h + 32, (j % 4) * 128:(j % 4 + 1) * 128], QB[:, s_, h, :], ktp[:, h, :], **tp(h))
                        if j % 4 == 3:
                            P.act(S_all[:, (j - 3) * 128:(j + 1) * 128], bS[:], AF.Copy, scale=0.125)
                    bS = nextbank()
                    for h in range(4):
                        P.mm(bS[32 * h:32 * h + 32, 0:8], QB[:, s_, h, :], SK[:, h, s_ * 8:(s_ + 1) * 8], **tp(h))
                    P.act(S_all[:, 8192:8200], bS[:, 0:8], AF.Copy, scale=0.125)
                    P.tt('dve', S_all[:, 8064:8200], S_all[:, 8064:8200], SBT, ALU.add)
                    P.red('dve', sm2[:, 4:5], S_all, ALU.max)
                    P.ts('dve', sm2[:, 5:6], sm2[:, 4:5], -1.0, None, op0=ALU.mult)
                    P.act(Pm, S_all, AF.Exp, bias=sm2[:, 5:6], scale=1.0, accum_out=sm2[:, 6:7])
                    P.recip(sm2[:, 7:8], sm2[:, 6:7])
                    for j in range(64):
                        vt = vtl[gi[0] % 3]
                        vb = vbl[gi[0] % 2]
                        ptp = PTp[gi[0] % 2]
                        gi[0] += 1
                        gather(vt, cache_v, s_ * 64 + j)
                        P.copy('dve', vb, vt)
                        bP = nextbank()
                        bPv = bP[:].bitcast(BF16)
                        P.tr(bPv[:, 0:128], Pm[:, j * 128:(j + 1) * 128], idb[:])
                        P.act(ptp, bPv[:, 0:128], AF.Copy)
                        for h in range(4):
                            P.mm(bO[32 * h:32 * h + 32, 0:128], ptp[:, 32 * h:32 * h + 32], vb[:, h * 128:(h + 1) * 128],
                                 start=(j == 0), stop=False, **tp(h))
                    bP = nextbank()
                    bPv = bP[:].bitcast(BF16)
                    P.tr(bPv[0:8, 0:128], Pm[:, 8192:8200], idb[:])
                    P.act(PTn[0:8, :], bPv[0:8, 0:128], AF.Copy)
                    for h in range(4):
                        P.mm(bO[32 * h:32 * h + 32, 0:128], PTn[0:8, 32 * h:32 * h + 32], SVs[s_][:, h * 128:(h + 1) * 128],
                             start=False, stop=True, **tp(h))
                    P.ts('dve', On, bO[:, 0:128], sm2[:, 7:8], None, op0=ALU.mult)
                    bC = nextbank()
                    P.mm(bC[0:32, 0:128], Dm, On)
                    P.act(sq[0:32, 0:128], bC[0:32, 0:128], AF.Square, accum_out=sm2[0:32, 8:9])
                    P.act(sm2[0:32, 9:10], sm2[0:32, 8:9], AF.Sqrt, bias=EPS, scale=1.0 / 128)
                    P.recip(sm2[0:32, 10:11], sm2[0:32, 9:10])
                    P.ts('dve', osb[0:32, :], bC[0:32, 0:128], sm2[0:32, 10:11], None, op0=ALU.mult)
                    bT = nextbank()
                    P.tr(bT[:, 0:32], osb[0:32, :], idf[0:32, 0:32])
                    P.ts('dve', SAO[:, :, s_ * 8:(s_ + 1) * 8], bT[:, 0:32].rearrange("p (h q) -> p h q", h=4), gsubc, None, op0=ALU.mult)
                nbanks[0] = 8
            else:
                P.memset('dve', SAO[:], 0.0)
            chunks = [SAO[:, h, :] for h in range(4)] + [SGS[:, g, :] for g in range(4)]
            P.memset('dve', LG[:, 16, :], 0.0)
            b2_block(16, 32, chunks, xs, G1s)

            if STAGE <= 5:
                raise _Stop()
            router(P, LG, COMB, RT)
            slot = [0]

            def eload(src2d_rows, ncols, kparts):
                v = ESLOT[slot[0] % 4].rearrange("p (k n) -> p k n", k=kparts)
                slot[0] += 1
                for k in range(kparts):
                    P.dma('pool', v[:, k, :], src2d_rows(k))
                return v

            groups = [(0, 512, 4), (512, 512, 4), (1024, 512, 4), (1536, 512, 4), (2048, 32, 1)]
            hi = [0]
            for e in range(16):
                wg = eload(lambda k: w_gate[e, k * 128:(k + 1) * 128, :], 512, 8)
                wu = eload(lambda k: w_up[e, k * 128:(k + 1) * 128, :], 512, 8)
                wd = eload(lambda k: w_down[e, k * 128:(k + 1) * 128, :], 1024, 4)
                for (c0, N, nb) in groups:
                    hb = he[hi[0] % 2]
                    hi[0] += 1
                    for fc in range(4):
                        bg = nextbank()
                        bu = nextbank()
                        for dk in range(8):
                            P.mm(bg[:, 0:N], wg[:, dk, fc * 128:(fc + 1) * 128], H2T[:, dk, c0:c0 + N], start=(dk == 0), stop=(dk == 7))
                        for dk in range(8):
                            P.mm(bu[:, 0:N], wu[:, dk, fc * 128:(fc + 1) * 128], H2T[:, dk, c0:c0 + N], start=(dk == 0), stop=(dk == 7))
                        P.act(sil[:, 0:N], bg[:, 0:N], AF.Silu)
                        P.tt('dve', hb[:, fc, 0:N], sil[:, 0:N], bu[:, 0:N], ALU.mult)
                    for b in range(nb):
                        blk = c0 // 128 + b
                        rows = 128 if N == 512 else 32
                        for half in range(2):
                            by = nextbank()
                            for fc in range(4):
                                P.mm(by[0:rows, :], hb[:, fc, b * 128:b * 128 + rows], wd[:, fc, half * 512:(half + 1) * 512],
                                     start=(fc == 0), stop=(fc == 3))
                            ya = yacc(blk)[0:rows, half * 512:(half + 1) * 512]
                            if e == 0:
                                P.ts('dve', ya, by[0:rows, :], COMB[0:rows, blk, e:e + 1], None, op0=ALU.mult)
                            else:
                                P.stt('dve', ya, by[0:rows, :], COMB[0:rows, blk, e:e + 1], ya, ALU.mult, ALU.add)

            gfin = W3[:, 8192:10240].bitcast(F32)
            P.dma('sp', gfin, gfin_in.to_broadcast([128, D]))
            for blk in range(17):
                rows = 128 if blk < 16 else 32
                Gt = G2p if blk < 16 else G2s
                xb = xt[blk % 2]
                P.dma('sp', xb[0:rows, :], x1scr[blk * 128:blk * 128 + rows, :])
                P.tt('dve', tmpf[0:rows, :], yacc(blk)[0:rows, :], Gt[0:rows, :], ALU.mult)
                P.tt('dve', x1b[0:rows, :], tmpf[0:rows, :], xb[0:rows, :], ALU.add)
                norm_rows(x1b[0:rows, :], rows, xnf[0:rows, :])
                P.tt('dve', tmpf[0:rows, :], xnf[0:rows, :], gfin[0:rows, :], ALU.mult)
                if blk < 16:
                    P.dma('sp', yp[blk * 128:(blk + 1) * 128, :], tmpf[:, :])
                else:
                    P.dma('sp', ys, tmpf[0:32, :])


      except _Stop:
        pass
      P.emit()
    return nc, P


def router(P, LG, COMB, RT):
    lg = LG[:, :, 0:4]
    le = LG[:, :, 4:20].rearrange("p b (g j) -> p b g j", g=4)
    gmax = RT[:, :, 0:1]
    P.red('dve', RT[:, :, 0], lg, ALU.max)
    gsel = RT[:, :, 1:5]
    P.tt('dve', gsel, lg, gmax.to_broadcast([128, 17, 4]), ALU.is_equal)
    P.tt('dve', RT[:, :, 5:9], lg, gmax.to_broadcast([128, 17, 4]), ALU.subtract)
    P.act(RT[:, :, 5:9], RT[:, :, 5:9], AF.Exp)
    P.red('dve', RT[:, :, 9], RT[:, :, 5:9], ALU.add)
    P.recip(RT[:, :, 9], RT[:, :, 9])
    gp = RT[:, :, 9:10]
    sel = RT[:, :, 10:14]
    for g in range(4):
        if g == 0:
            P.tt('dve', sel, le[:, :, 0, :], gsel[:, :, 0:1].to_broadcast([128, 17, 4]), ALU.mult)
        else:
            P.tt('dve', RT[:, :, 5:9], le[:, :, g, :], gsel[:, :, g:g + 1].to_broadcast([128, 17, 4]), ALU.mult)
            P.tt('dve', sel, sel, RT[:, :, 5:9], ALU.add)
    m1 = RT[:, :, 14:15]
    P.red('dve', RT[:, :, 14], sel, ALU.max)
    mask1 = RT[:, :, 15:19]
    P.tt('dve', mask1, sel, m1.to_broadcast([128, 17, 4]), ALU.is_equal)
    sel2 = RT[:, :, 5:9]
    P.stt('dve', sel2, mask1, -1e30, sel, ALU.mult, ALU.add)
    m2 = RT[:, :, 19:20]
    P.red('dve', RT[:, :, 19], sel2, ALU.max)
    mask2 = RT[:, :, 20:24]
    P.tt('dve', mask2, sel2, m2.to_broadcast([128, 17, 4]), ALU.is_equal)
    d = RT[:, :, 0:1]
    P.tt('dve', d, m2, m1, ALU.subtract)
    P.act(d, d, AF.Exp)
    w1 = RT[:, :, 14:15]
    P.ts('dve', w1, d, 1.0, None, op0=ALU.add)
    P.recip(RT[:, :, 14], RT[:, :, 14])
    w2 = RT[:, :, 19:20]
    P.tt('dve', w2, d, w1, ALU.mult)
    P.tt('dve', w1, w1, gp, ALU.mult)
    P.tt('dve', w2, w2, gp, ALU.mult)
    cig = RT[:, :, 5:9]
    P.tt('dve', cig, mask1, w1.to_broadcast([128, 17, 4]), ALU.mult)
    P.tt('dve', mask2, mask2, w2.to_broadcast([128, 17, 4]), ALU.mult)
    P.tt('dve', cig, cig, mask2, ALU.add)
    cv = COMB.rearrange("p b (g j) -> p b g j", g=4) if False else None
    for g in range(4):
        P.tt('dve', COMB[:, :, g * 4:(g + 1) * 4], cig, gsel[:, :, g:g + 1].to_broadcast([128, 17, 4]), ALU.mult)


def sample_attention(P, nc, L):
    raise NotImplementedError


def _t5_bucket_np(n):
    n = np.maximum(n, 0)
    nf = np.maximum(n, 1).astype(np.float32)
    large = 16 + (np.log(nf / 16) / np.log(128 / 16) * 16).astype(np.int32)
    large = np.minimum(large, 31)
    return np.where(n < 16, n, large)


_CACHE = {}
DO_SAMPLE = True


def kernel(x_prompt, x_sample, c_prompt, c_sample, cache_k, cache_v, page_table,
           w_ada, b_ada, w_in, w_o, lam_q1, lam_k1, lam_q2, lam_k2, g_subln, rel_bias,
           g_sg_ln, b_sg_ln, w_s, b_s, w_rg, b_rg, w_re, b_re, w_gate, w_up, w_down, g_final):
    f32 = np.float32
    A = lambda a: np.ascontiguousarray(np.asarray(a))
    x_prompt = A(x_prompt); x_sample = A(x_sample)
    rel_bias = A(rel_bias)
    key = ('nc', DO_SAMPLE)
    if key not in _CACHE:
        _CACHE[key] = build_program(DO_SAMPLE)
    nc, P = _CACHE[key]

    kk = np.arange(128)[:, None]
    qq = np.arange(128)[None, :]
    dist_diag = qq - kk
    dist_sub = 128 + qq - kk
    dist_far = np.full((128, 128), 100000)
    bk_diag = _t5_bucket_np(dist_diag)
    bk_sub = _t5_bucket_np(dist_sub)
    bk_far = _t5_bucket_np(dist_far)
    m_diag = (dist_diag >= 0).astype(f32)
    m_one = np.ones((128, 128), f32)
    m_zero = np.zeros((128, 128), f32)
    tril = (np.arange(128)[:, None] <= np.arange(128)[None, :]).astype(f32)
    ii = np.arange(32)
    trilS = ((ii[:, None] // 8 == ii[None, :] // 8) & (ii[:, None] <= ii[None, :])).astype(f32)
    sel = np.zeros((5, 160), f32)
    sel[0, 0:128] = 1.0
    for s in range(4):
        sel[1 + s, 128 + s * 8:128 + (s + 1) * 8] = 1.0
    ident = np.eye(128).astype(ml_dtypes.bfloat16)
    identf = np.eye(128, dtype=f32)

    w_s0 = A(w_s)[0]
    wsT = np.ascontiguousarray(np.transpose(w_s0, (2, 0, 1)).reshape(128, 512))
    wsS = np.zeros((32, 4, 32), f32)
    for s in range(4):
        wsS[s * 8:(s + 1) * 8, :, s * 8:(s + 1) * 8] = np.transpose(w_s0[:, :8, :8], (2, 0, 1))
    wsS = wsS.reshape(32, 128)
    bs0 = A(b_s)[0]
    bsS = np.ascontiguousarray(bs0[:, np.arange(32) % 8]).reshape(1, 128)
    wr = np.ascontiguousarray(np.concatenate([A(w_rg)[0], A(w_re)[0]], axis=1))
    br = np.ascontiguousarray(np.concatenate([A(b_rg)[0], A(b_re)[0]])[None])
    lam4 = np.ascontiguousarray(np.concatenate([A(lam_q1)[0], A(lam_k1)[0], A(lam_q2)[0], A(lam_k2)[0]])[None])

    shared = dict(
        w_ada=A(w_ada)[0], b_ada=A(b_ada), w_in=A(w_in)[0], w_o=A(w_o)[0], lam4=lam4,
        gsub=np.ascontiguousarray(A(g_subln).reshape(128, 1)),
        b31=np.ascontiguousarray(rel_bias[31:32, :]),
        gsg=A(g_sg_ln).reshape(1, 512), bsg=A(b_sg_ln).reshape(1, 512),
        wsT=wsT, tril=tril, wsS=wsS, trilS=trilS, bs=bs0.reshape(1, 512), bsS=bsS,
        wr=wr, br=br, w_gate=A(w_gate)[0], w_up=A(w_up)[0], w_down=A(w_down)[0],
        gfin=A(g_final).reshape(1, D), ident=ident, identf=identf, sel=sel,
    )
    if DO_SAMPLE:
        shared['cache_k'] = A(cache_k).reshape(2560 * 128, 512)
        shared['cache_v'] = A(cache_v).reshape(2560 * 128, 512)
        pp = np.arange(128)
        hh = pp // 32
        q_of = (pp % 32) % 8
        cc = np.arange(136)
        dist_s = np.where(cc[None, :] < 128, 128 + q_of[:, None] - cc[None, :], q_of[:, None] - (cc[None, :] - 128))
        shared['sbias'] = np.ascontiguousarray(rel_bias[_t5_bucket_np(dist_s), hh[:, None]]).astype(f32)
        shared['smask'] = np.where(dist_s >= 0, 0.0, NEG).astype(f32)
        shared['sb31'] = np.ascontiguousarray(rel_bias[31, hh][:, None]).astype(f32)
        dsel = np.zeros((128, 64), f32)
        for h in range(4):
            for q in range(8):
                dsel[h * 32 + q, h * 8 + q] = 1.0
                dsel[h * 32 + 8 + q, 32 + h * 8 + q] = 1.0
        shared['dsel'] = dsel
        shared['pidx'] = np.arange(128, dtype=f32).reshape(128, 1)

    in_maps = []
    for c in range(NCORES):
        b, cpar = c // 2, c % 2
        order = []
        for g in range(8):
            order += [_own_block(cpar, 2 * g), _own_block(cpar, 2 * g + 1),
                      _partner_block(cpar, 2 * g), _partner_block(cpar, 2 * g + 1)]
        xb = x_prompt[b].reshape(32, 128, D)
        xpc = np.ascontiguousarray(xb[order].reshape(4096, D))
        cinc = np.ascontiguousarray(np.concatenate([A(c_prompt)[b:b + 1], A(c_sample)[4 * c:4 * c + 4]], axis=0))
        bks, mks = [bk_diag], [m_diag]
        for tau in (0, 1):
            second = (tau + cpar) % 2 == 1
            bks.append(bk_sub if second else bk_far)
            mks.append(m_one if second else m_zero)
        for tau in (0, 1):
            second = (tau + cpar) % 2 == 1
            bks.append(bk_far if second else bk_sub)
            mks.append(m_one)
        bg = np.stack([rel_bias[bk] for bk in bks], axis=0)
        bg = np.ascontiguousarray(np.transpose(bg, (1, 0, 3, 2)).reshape(128, 5 * 4 * 128)).astype(f32)
        mg = np.ascontiguousarray(np.transpose(np.stack(mks, 0), (1, 0, 2)).reshape(128, 5 * 128)).astype(f32)
        m = dict(shared)
        m.update(xp=xpc, xs=np.ascontiguousarray(x_sample[4 * c:4 * c + 4].reshape(32, D)), cin=cinc, biasg=bg, maskg=mg)
        if DO_SAMPLE:
            m['pt'] = np.ascontiguousarray(A(page_table)[4 * c:4 * c + 4].reshape(1, 256)).astype(np.int32)
        in_maps.append(m)

    ncr = int(os.environ.get('MK_CORES', str(NCORES)))
    res = run_bass_kernel_spmd(nc, in_maps[:ncr], core_ids=list(range(ncr)))
    R = list(res.results) + [res.results[0]] * (NCORES - ncr)
    y_prompt = np.zeros((4, 4096, D), f32)
    kp = np.zeros((4, 1, 4096, 4, 128), f32)
    vp = np.zeros((4, 1, 4096, 4, 128), f32)
    y_sample = np.zeros((32, 8, D), f32)
    ks = np.zeros((32, 1, 8, 4, 128), f32)
    vs = np.zeros((32, 1, 8, 4, 128), f32)
    sg = np.zeros((32, 1, 8, 4, 128), f32)
    for c in range(NCORES):
        b, cpar = c // 2, c % 2
        r = R[c]
        for t in range(16):
            gblk = _own_block(cpar, t)
            y_prompt[b, gblk * 128:(gblk + 1) * 128] = r['yp'][t * 128:(t + 1) * 128]
            kp[b, 0, gblk * 128:(gblk + 1) * 128] = r['kp'][t * 128:(t + 1) * 128].reshape(128, 4, 128)
            vp[b, 0, gblk * 128:(gblk + 1) * 128] = r['vp'][t * 128:(t + 1) * 128].reshape(128, 4, 128)
        y_sample[4 * c:4 * c + 4] = r['ys'].reshape(4, 8, D)
        ks[4 * c:4 * c + 4, 0] = r['ks'].reshape(4, 8, 4, 128)
        vs[4 * c:4 * c + 4, 0] = r['vso'].reshape(4, 8, 4, 128)
        sg[4 * c:4 * c + 4, 0] = r['sgv'].reshape(4, 8, 4, 128)
    return (y_prompt, y_sample, kp, vp, ks, vs, sg)
```

```python
import numpy as np
import concourse.bass as bass
import concourse.mybir as mybir

F32 = mybir.dt.float32
BF16 = mybir.dt.bfloat16
I32 = mybir.dt.int32
AF = mybir.ActivationFunctionType
ALU = mybir.AluOpType
AX = mybir.AxisListType

_ESZ = {}


def _esz(dt):
    s = str(dt)
    if s not in _ESZ:
        if '32' in s:
            _ESZ[s] = 4
        elif '16' in s:
            _ESZ[s] = 2
        elif '64' in s:
            _ESZ[s] = 8
        else:
            _ESZ[s] = 1
    return _ESZ[s]


def box(ap):
    t = ap.tensor
    name = t.name
    esz = _esz(ap.dtype)
    dims = ap.ap
    off = ap.offset
    if not isinstance(off, int):
        return (name, 0, 1 << 30, 0, 1 << 40)
    tn = type(t).__name__
    if tn.startswith('PSum'):
        return ('PSUM:' + name, 0, 128, 0, 1 << 20)
    if tn.startswith('SB'):
        pstep, pcnt = dims[0]
        if pstep == 0:
            pstep = 1 << 40
        p0 = off // pstep + getattr(t, 'base_partition', 0) if pstep < (1 << 40) else 0
        f0 = off % pstep if pstep < (1 << 40) else off
        ext = sum((c - 1) * abs(s) for s, c in dims[1:])
        return (name, p0, p0 + pcnt, f0 * esz, (f0 + ext + 1) * esz)
    ext = sum((c - 1) * abs(s) for s, c in dims)
    return (name, 0, 1, off * esz, (off + ext + 1) * esz)


class Op:
    __slots__ = ('id', 'eng', 'kind', 'fn', 'deps', 'marked', 'rank', 'sem', 'semval', 'waits')

    def __init__(self, id, eng, kind, fn):
        self.id = id
        self.eng = eng
        self.kind = kind
        self.fn = fn
        self.deps = {}
        self.marked = False
        self.rank = 0
        self.sem = None
        self.semval = 0
        self.waits = []


EPOCH = 2000


class Prog:
    def __init__(self, nc, n_dma_sems=40):
        self.nc = nc
        self.ops = []
        self.hist = {}
        self.n_dma_sems = n_dma_sems
        self.track_off = set()

    def _add(self, eng, kind, fn, reads, writes):
        op = Op(len(self.ops), eng, kind, fn)
        self.ops.append(op)
        for ap in reads:
            if ap is None or isinstance(ap, (int, float)):
                continue
            self._access(op, box(ap), False)
        for ap in writes:
            if ap is None:
                continue
            self._access(op, box(ap), True)
        return op

    def _access(self, op, b, is_write):
        name, plo, phi, flo, fhi = b
        if name in self.track_off:
            return
        h = self.hist.setdefault(name, [])
        keep = []
        for e in h:
            eplo, ephi, eflo, efhi, eid, ew = e
            if eid == op.id:
                keep.append(e)
                continue
            ov = not (ephi <= plo or phi <= eplo or efhi <= flo or fhi <= eflo)
            if ov and name.startswith('PSUM:') and not ew and not is_write and self.ops[eid].eng != op.eng:
                op.deps.setdefault(eid, 'rar')
            if ov:
                if ew or is_write:
                    typ = 'raw' if (ew and not is_write) else ('waw' if ew else 'war')
                    old = op.deps.get(eid)
                    if old is None or typ == 'raw':
                        op.deps[eid] = typ
                if is_write and plo <= eplo and ephi <= phi and flo <= eflo and efhi <= fhi:
                    continue
            keep.append(e)
        if not is_write:
            if op.kind == 'c':
                keep2 = []
                for e in keep:
                    if (not e[5]) and e[0] == plo and e[1] == phi and e[2] == flo and e[3] == fhi:
                        o = self.ops[e[4]]
                        if o.kind == 'c' and o.eng == op.eng:
                            continue
                    keep2.append(e)
                keep = keep2
        keep.append((plo, phi, flo, fhi, op.id, is_write))
        self.hist[name] = keep

    def mm(self, out, lhsT, rhs, start=True, stop=True, **kw):
        return self._add('pe', 'c', lambda e: e.matmul(out, lhsT, rhs, start=start, stop=stop, **kw),
                         [lhsT, rhs], [out])

    def tr(self, out, in_, ident):
        return self._add('pe', 'c', lambda e: e.transpose(out, in_, ident), [in_, ident], [out])

    def act(self, out, in_, func, bias=0.0, scale=1.0, accum_out=None):
        rd = [in_]
        if not isinstance(bias, (int, float)):
            rd.append(bias)
        if not isinstance(scale, (int, float)):
            rd.append(scale)
        kw = {}
        if accum_out is not None:
            kw['accum_out'] = accum_out
        return self._add('act', 'c', lambda e: e.activation(out, in_, func, bias=bias, scale=scale, **kw),
                         rd, [out, accum_out])

    def ts(self, eng, out, in0, s1, s2=None, op0=ALU.mult, op1=None, accum_out=None):
        rd = [in0]
        if not isinstance(s1, (int, float)):
            rd.append(s1)
        if s2 is not None and not isinstance(s2, (int, float)):
            rd.append(s2)
        kw = {}
        if op1 is not None:
            kw['op1'] = op1
        if accum_out is not None:
            kw['accum_out'] = accum_out
        return self._add(eng, 'c', lambda e: e.tensor_scalar(out, in0, s1, s2, op0, **kw), rd, [out, accum_out])

    def tt(self, eng, out, in0, in1, op):
        return self._add(eng, 'c', lambda e: e.tensor_tensor(out, in0, in1, op), [in0, in1], [out])

    def stt(self, eng, out, in0, scalar, in1, op0, op1):
        rd = [in0, in1]
        if not isinstance(scalar, (int, float)):
            rd.append(scalar)
        return self._add(eng, 'c', lambda e: e.scalar_tensor_tensor(out, in0, scalar, in1, op0, op1), rd, [out])

    def copy(self, eng, out, in_):
        if eng == 'act':
            return self._add('act', 'c', lambda e: e.copy(out, in_), [in_], [out])
        return self._add(eng, 'c', lambda e: e.tensor_copy(out, in_), [in_], [out])

    def red(self, eng, out, in_, op, axis=AX.X):
        return self._add(eng, 'c', lambda e: e.tensor_reduce(out, in_, axis, op), [in_], [out])

    def recip(self, out, in_):
        return self._add('dve', 'c', lambda e: e.reciprocal(out, in_), [in_], [out])

    def memset(self, eng, ap, val):
        return self._add(eng, 'c', lambda e: e.memset(ap, val), [], [ap])

    def dma(self, q, out, in_, extra_reads=(), **kw):
        kind = 'sw' if q == 'pool' else 'dma'
        return self._add(q, kind, lambda e: e.dma_start(out=out, in_=in_, **kw), [in_] + list(extra_reads), [out])

    def custom(self, eng, kind, fn, reads, writes):
        return self._add(eng, kind, fn, reads, writes)

    def emit(self):
        nc = self.nc
        ops = self.ops
        n_hw = self.n_dma_sems
        n_sw = 8
        last_on_sem = {}
        di = {'dma': 0, 'sw': 0}
        for op in ops:
            if op.kind in ('dma', 'sw'):
                if op.kind == 'dma':
                    s = di['dma'] % n_hw
                else:
                    s = n_hw + di['sw'] % n_sw
                di[op.kind] += 1
                op.sem = s
                prev = last_on_sem.get(s)
                op.semval = (prev.semval if prev else 0) + 16
                if prev is not None:
                    op.deps.setdefault(prev.id, 'waw')
                last_on_sem[s] = op

        def skip(p, op, typ):
            if p.kind != 'c':
                return False
            if p.eng == 'pe' and op.eng == 'pe' and op.kind == 'c':
                return True
            return False

        for op in ops:
            for d, typ in op.deps.items():
                p = ops[d]
                if p.kind == 'c' and not skip(p, op, typ):
                    p.marked = True
        cnt = {}
        for op in ops:
            if op.kind == 'c' and op.marked:
                cnt[op.eng] = cnt.get(op.eng, 0) + 1
                op.rank = cnt[op.eng]
        engs = ['pe', 'act', 'dve', 'pool', 'sp']
        waited = {e: {} for e in engs}
        nwaits = 0
        for op in ops:
            need = {}
            for d, typ in op.deps.items():
                p = ops[d]
                if p.kind == 'c':
                    if skip(p, op, typ):
                        continue
                    key = ('e', p.eng, (p.rank - 1) // EPOCH)
                    val = (p.rank - 1) % EPOCH + 1
                else:
                    key = ('d', p.sem)
                    val = p.semval
                if need.get(key, 0) < val:
                    need[key] = val
            w = waited[op.eng]
            for key, val in need.items():
                if w.get(key, 0) >= val:
                    continue
                w[key] = val
                op.waits.append((key, val))
                nwaits += 1
        self.stats = dict(n_ops=len(ops), n_waits=nwaits, marked=dict(cnt), n_dma=dict(di))
        import contextlib
        with contextlib.ExitStack() as st:
            esem = {}
            for e in ['pe', 'act', 'dve', 'pool']:
                for k in range((cnt.get(e, 0) + EPOCH - 1) // EPOCH + 1):
                    esem[(e, k)] = st.enter_context(nc.semaphore('es_%s_%d' % (e, k)))
            dsem = [st.enter_context(nc.semaphore('ds_%d' % i)) for i in range(n_hw + n_sw)]
            block = st.enter_context(nc.Block())
            per = {e: [o for o in ops if o.eng == e] for e in engs}

            def run(e, eng):
                for op in per[e]:
                    for key, val in op.waits:
                        sem = esem[(key[1], key[2])] if key[0] == 'e' else dsem[key[1]]
                        eng.wait_ge(sem, val)
                    ins = op.fn(eng)
                    if op.kind in ('dma', 'sw'):
                        ins.then_inc(dsem[op.sem], 16)
                    elif op.marked:
                        ins.then_inc(esem[(e, (op.rank - 1) // EPOCH)], 1)
                if e == 'sp':
                    for s_, o in last_on_sem.items():
                        eng.wait_ge(dsem[s_], o.semval)

            @block.tensor
            def _(eng):
                run('pe', eng)

            @block.scalar
            def _(eng):
                run('act', eng)

            @block.vector
            def _(eng):
                run('dve', eng)

            @block.gpsimd
            def _(eng):
                run('pool', eng)

            @block.sync
            def _(eng):
                run('sp', eng)

import contextlib
import ml_dtypes
from concourse.bass_utils import run_bass_kernel_spmd

D = 1024
EPS = 1e-6
LAM_INIT = 0.2
NCORES = 8
NEG = -30000.0


def _own_block(cpar, t):
    return 2 * t + ((t + cpar) % 2)


def _partner_block(cpar, t):
    return 2 * t + 1 - ((t + cpar) % 2)


def _pblk(t, pos):
    return (t // 2) * 4 + (t % 2) + 2 * pos


import os
STAGE = int(os.environ.get('MK_STAGE', '9'))
SUB = int(os.environ.get('MK_SUB', '99'))


def _chk(n):
    if SUB <= n:
        raise _Stop()


class _Stop(Exception):
    pass


def build_program(do_sample=True):
    nc = bass.Bass("TRN2", target_bir_lowering=False)

    def din(name, shape, dt=F32):
        return nc.dram_tensor(name, list(shape), dt, kind="ExternalInput").ap()

    def dout(name, shape, dt=F32):
        return nc.dram_tensor(name, list(shape), dt, kind="ExternalOutput").ap()

    xp = din("xp", [4096, D])
    xs = din("xs", [32, D])
    cin = din("cin", [5, D])
    w_ada = din("w_ada", [D, 6 * D])
    b_ada = din("b_ada", [1, 6 * D])
    w_in = din("w_in", [D, 2560])
    w_o = din("w_o", [D, D])
    lam4 = din("lam4", [1, 256])
    gsub_in = din("gsub", [128, 1])
    biasg = din("biasg", [128, 5 * 4 * 128])
    maskg = din("maskg", [128, 5 * 128])
    b31_in = din("b31", [1, 4])
    gsg_in = din("gsg", [1, 512])
    bsg_in = din("bsg", [1, 512])
    wsT_in = din("wsT", [128, 4 * 128])
    tril_in = din("tril", [128, 128])
    wsS_in = din("wsS", [32, 4 * 32])
    trilS_in = din("trilS", [32, 32])
    bs_in = din("bs", [1, 512])
    bsS_in = din("bsS", [1, 128])
    wr_in = din("wr", [D, 20])
    br_in = din("br", [1, 20])
    w_gate = din("w_gate", [16, D, 512])
    w_up = din("w_up", [16, D, 512])
    w_down = din("w_down", [16, 512, D])
    gfin_in = din("gfin", [1, D])
    ident_in = din("ident", [128, 128], BF16)
    identf_in = din("identf", [128, 128])
    sel_in = din("sel", [5, 160])
    if do_sample:
        cache_k = din("cache_k", [2560 * 128, 512])
        cache_v = din("cache_v", [2560 * 128, 512])
        pt_in = din("pt", [1, 256], I32)
        sbias_in = din("sbias", [128, 136])
        smask_in = din("smask", [128, 136])
        sb31_in = din("sb31", [128, 1])
        dsel_in = din("dsel", [128, 64])
        pidx_in = din("pidx", [128, 1])

    yp = dout("yp", [2048, D])
    ys = dout("ys", [32, D])
    kp = dout("kp", [2048, 512])
    vp = dout("vp", [2048, 512])
    ks = dout("ks", [32, 512])
    vso = dout("vso", [32, 512])
    sgv = dout("sgv", [32, 512])
    x1scr = nc.dram_tensor("x1scr", [2176, D], F32, kind="Internal").ap()

    P = Prog(nc)
    with contextlib.ExitStack() as st:
      try:
            def sb(name, shape, dt):
                return st.enter_context(nc.sbuf_tensor("s_" + name, list(shape), dt))

            A1 = sb("A1", [128, 52224], BF16)
            A2 = sb("A2", [128, 20480], BF16)
            idb = sb("idb", [128, 128], BF16)
            idf = sb("idf", [128, 128], F32)
            onesb = sb("onesb", [128, 128], BF16)
            modT = sb("modT", [128, 48, 5], F32)
            G1p = sb("G1p", [128, D], BF16)
            G2p = sb("G2p", [128, D], BF16)
            G1s = sb("G1s", [32, D], BF16)
            G2s = sb("G2s", [32, D], BF16)
            EB = sb("EB", [128, 5, 4, 128], BF16)
            bsSB = sb("bsSB", [128, 128], F32)
            wts = sb("wts", [128, 4, 128], BF16)
            wtsS = sb("wtsS", [32, 4, 32], BF16)
            wrt = sb("wrt", [128, 8, 20], F32)
            brB = sb("brB", [128, 20], F32)
            sml = sb("sml", [128, 64], F32)
            ssb = sb("ssb", [128, 64], F32)
            xt = [sb("xt%d" % i, [128, D], F32) for i in range(2)]
            sq = sb("sq", [128, D], BF16)
            xn = [sb("xn%d" % i, [128, D], BF16) for i in range(4)]
            W3 = sb("W3", [128, 13312], BF16)

            QT = A1[:, 0:8192].rearrange("p (h t) -> p h t", h=4)
            SGT = A1[:, 8192:16384].rearrange("p (g t) -> p g t", g=4)
            KT = A1[:, 16384:32768].rearrange("p (h t) -> p h t", h=4)
            VS = A1[:, 32768:49152].rearrange("p (b c) -> p b c", b=32)
            TAIL = A1[:, 49152:52224].bitcast(F32)
            gsgB = TAIL[:, 0:512]
            bsgB = TAIL[:, 512:1024]
            bsB = TAIL[:, 1024:1536]
            H2T = A1[:, 16384:16384 + 8 * 2176].rearrange("p (k t) -> p k t", k=8)
            yaccA = A1[:, 0:16384].bitcast(F32).rearrange("p (b d) -> p b d", d=D)
            yaccB = A1[:, 33792:33792 + 18432].bitcast(F32).rearrange("p (b d) -> p b d", d=D)

            def yacc(blk):
                return yaccA[:, blk, :] if blk < 8 else yaccB[:, blk - 8, :]

            WIN = A2[:, 0:20480].rearrange("p (k n) -> p k n", k=8)
            WO = A2[:, 0:8192].rearrange("p (k n) -> p k n", k=8)
            ESLOT = [A2[:, i * 4096:(i + 1) * 4096] for i in range(4)]
            RTALL = A2[:, 16384:16384 + 2048].bitcast(F32)
            LG = RTALL[:, 0:340].rearrange("p (b n) -> p b n", b=17)
            COMB = RTALL[:, 340:612].rearrange("p (b n) -> p b n", b=17)
            RT = RTALL[:, 612:1020].rearrange("p (b n) -> p b n", b=17)
            hT = W3[:, 0:4096].rearrange("p (k t) -> p k t", k=8)
            kst = W3[:, 4096:5120].bitcast(F32)
            vst = W3[:, 5120:6144].bitcast(F32)
            k16 = W3[:, 6144:6656]
            uT = W3[:, 6656:7680].rearrange("p (g t) -> p g t", g=4)
            vln = W3[:, 7680:8704].bitcast(F32)
            vlb = W3[:, 8704:9216]
            sqv = W3[:, 9216:10240].bitcast(F32)
            gtmp = W3[:, 10240:11264].bitcast(F32)
            wach = [W3[:, 4096 + i * 2048: 4096 + (i + 1) * 2048].rearrange("p (k n) -> p k n", k=8) for i in range(2)]
            mrow = W3[:, 0:512].bitcast(F32)
            bac = W3[:, 512:1024].bitcast(F32)
            PT = [W3[:, 11264 + i * 512: 11264 + (i + 1) * 512] for i in range(4)]
            ep_rl = W3[:, 0:1024].bitcast(F32)
            ep_on0 = W3[:, 1024:2048].bitcast(F32)
            ep_on1 = W3[:, 2048:3072].bitcast(F32)
            ep_o = W3[:, 3072:4096].bitcast(F32)
            ep_sq = W3[:, 4096:4608]
            ep_r = W3[:, 4608:5632].bitcast(F32)
            h2Tf = W3[:, 0:2048].bitcast(F32).rearrange("p (k t) -> p k t", k=8)
            x1b = W3[:, 2048:4096].bitcast(F32)
            xnf = W3[:, 4096:6144].bitcast(F32)
            tmpf = W3[:, 6144:8192].bitcast(F32)
            he = [W3[:, 8192 + i * 2048: 8192 + (i + 1) * 2048].rearrange("p (f t) -> p f t", f=4) for i in range(2)]
            sil = W3[:, 12288:13312].bitcast(F32)

            pb = [st.enter_context(nc.psum_tensor("pb%d" % i, [128, 512], F32)) for i in range(8)]
            pbi = [0]

            nbanks = [8]

            def nextbank():
                b = pb[pbi[0] % nbanks[0]]
                pbi[0] += 1
                return b

            ssi = [0]

            def sscols(n=4):
                i = ssi[0] % 16
                ssi[0] += 1
                return ssb[:, i * 4:(i * 4 + n)]

            evi = [0]

            def evac_eng():
                evi[0] += 1
                return 'dve' if evi[0] % 2 else 'act'

            def affine(eng, out, in_, s_ap, b_ap):
                if eng == 'act':
                    P.act(out, in_, AF.Identity, bias=b_ap, scale=s_ap)
                else:
                    P.ts(eng, out, in_, s_ap, b_ap, op0=ALU.mult, op1=ALU.add)

            def pcopy(eng, out, in_):
                if eng == 'act':
                    P.act(out, in_, AF.Copy)
                else:
                    P.copy(eng, out, in_)

            P.dma('sp', idb[:], ident_in)
            P.dma('sp', idf[:], identf_in)
            P.memset('dve', onesb[:], 1.0)
            P.dma('sp', gsgB, gsg_in.to_broadcast([128, 512]))
            P.dma('sp', bsgB, bsg_in.to_broadcast([128, 512]))
            P.dma('sp', bsB, bs_in.to_broadcast([128, 512]))
            P.dma('sp', bsSB[:], bsS_in.to_broadcast([128, 128]))
            P.dma('sp', brB[:], br_in.to_broadcast([128, 20]))
            P.dma('sp', wrt[:], wr_in.rearrange("(k p) n -> p k n", p=128))
            P.dma('sp', sml[:, 8:9], gsub_in)
            P.dma('sp', sml[:, 12:16], b31_in.to_broadcast([128, 4]))
            lamt = gtmp[:, 0:256]
            P.dma('sp', lamt, lam4.to_broadcast([128, 256]))
            P.tt('dve', sqv[:, 0:64], lamt[:, 0:64], lamt[:, 64:128], ALU.mult)
            P.tt('dve', sqv[:, 64:128], lamt[:, 128:192], lamt[:, 192:256], ALU.mult)
            P.red('dve', sml[:, 0:2], sqv[:, 0:128].rearrange("p (a b) -> p a b", a=2), ALU.add)
            P.act(sml[:, 2:4], sml[:, 0:2], AF.Exp)
            P.tt('dve', sml[:, 4:5], sml[:, 3:4], sml[:, 2:3], ALU.subtract)
            P.ts('dve', sml[:, 5:6], sml[:, 4:5], -LAM_INIT, None, op0=ALU.add)
            neglam = sml[:, 5:6]
            P.ts('dve', sml[:, 9:10], sml[:, 8:9], 1.0 - LAM_INIT, None, op0=ALU.mult)
            gsubc = sml[:, 9:10]
            P.dma('sp', sqv[:, 0:512], wsT_in)
            P.dma('sp', gtmp[:, 0:128], tril_in)
            for g in range(4):
                P.tt('dve', wts[:, g, :], sqv[:, g * 128:(g + 1) * 128], gtmp[:, 0:128], ALU.mult)
            P.dma('sp', vln[0:32, 0:128], wsS_in)
            P.dma('sp', vln[0:32, 128:160], trilS_in)
            for g in range(4):
                P.tt('dve', wtsS[:, g, :], vln[0:32, g * 32:(g + 1) * 32], vln[0:32, 128:160], ALU.mult)

            cint = xt[0][0:5, :]
            P.dma('sp', cint, cin)
            scs = xt[1][0:5, :]
            P.act(scs, cint, AF.Silu)
            b0 = nextbank()
            for kc in range(8):
                P.tr(b0[:, kc * 5:(kc + 1) * 5], scs[:, kc * 128:(kc + 1) * 128], idf[0:5, 0:5])
            scT = sq[:, 0:40].rearrange("p (k r) -> p k r", k=8)
            P.copy('dve', scT, b0[:, 0:40].rearrange("p (k r) -> p k r", k=8))
            selt = sml[0:5, 16:16 + 0]
            selsb = sb("selsb", [5, 160], F32)
            P.dma('sp', selsb[:], sel_in)
            for n in range(24):
                wa = wach[n % 2]
                P.dma('pool', wa, w_ada[:, n * 256:(n + 1) * 256].rearrange("(k p) n -> p k n", p=128))
                P.dma('sp', bac[0:5, 0:256], b_ada[:, n * 256:(n + 1) * 256].to_broadcast([5, 256]))
                bk = nextbank()
                for kc in range(8):
                    P.mm(bk[0:5, 0:256], scT[:, kc, :], wa[:, kc, :], start=(kc == 0), stop=(kc == 7))
                P.tt('dve', mrow[0:5, 0:256], bk[0:5, 0:256], bac[0:5, 0:256], ALU.add)
                bk2 = nextbank()
                for j in range(2):
                    P.tr(bk2[:, j * 5:(j + 1) * 5], mrow[0:5, j * 128:(j + 1) * 128], idf[0:5, 0:5])
                P.copy('dve', modT[:, n * 2:(n + 1) * 2, :], bk2[:, 0:10].rearrange("p (k r) -> p k r", k=2))
                part = n // 4
                if part in (2, 5):
                    Gp, Gs = (G1p, G1s) if part == 2 else (G2p, G2s)
                    c0 = (n % 4) * 256
                    bk3 = nextbank()
                    P.mm(bk3[:, 0:256], selsb[0:5, 0:128], mrow[0:5, 0:256])
                    P.copy('dve', Gp[:, c0:c0 + 256], bk3[:, 0:256])
                    bk4 = nextbank()
                    P.mm(bk4[0:32, 0:256], selsb[0:5, 128:160], mrow[0:5, 0:256])
                    P.copy('dve', Gs[:, c0:c0 + 256], bk4[0:32, 0:256])
            for ch0 in (8, 32):
                P.ts('dve', modT[:, ch0:ch0 + 8, :], modT[:, ch0:ch0 + 8, :], 1.0, None, op0=ALU.add)

            ebsrc = [xt[0][:, :], xt[1][:, :]]
            P.dma('sp', xt[0][:, 0:1024], biasg[:, 0:1024])
            P.dma('sp', xt[1][:, 0:1024], biasg[:, 1024:2048])
            P.dma('sp', xnf[:, 0:512], biasg[:, 2048:2560])
            P.dma('sp', tmpf[:, 0:640], maskg)
            P.ts('dve', sml[:, 16:20], sml[:, 12:16], -1.0, None, op0=ALU.mult)
            for i in range(5):
                for h in range(4):
                    col = (i * 4 + h) * 128
                    src = (xt[0][:, col:col + 128] if col < 1024 else
                           xt[1][:, col - 1024:col - 1024 + 128] if col < 2048 else xnf[:, col - 2048:col - 2048 + 128])
                    P.act(x1b[:, 0:128], src, AF.Exp, bias=sml[:, 16 + h:17 + h], scale=1.0)
                    P.tt('dve', EB[:, i, h, :], x1b[:, 0:128], tmpf[:, i * 128:(i + 1) * 128], ALU.mult)

            if STAGE <= 1:
                raise _Stop()
            for k in range(8):
                P.dma('pool', WIN[:, k, :], w_in[k * 128:(k + 1) * 128, :])

            def norm_rows(x_ap, rows, out_ap):
                s = sscols(4)
                P.act(sq[0:rows, :], x_ap, AF.Square, accum_out=s[0:rows, 0:1])
                P.act(s[0:rows, 1:2], s[0:rows, 0:1], AF.Sqrt, bias=EPS, scale=1.0 / D)
                P.recip(s[0:rows, 2:3], s[0:rows, 1:2])
                P.ts('dve', out_ap, x_ap, s[0:rows, 2:3], None, op0=ALU.mult)

            def ln_groups(ps, rows, out_f32, out_bf):
                s = sscols(4)
                s2 = sscols(4)
                s3 = sscols(4)
                s4 = sscols(4)
                pv = ps.rearrange("p (g c) -> p g c", g=4)
                P.red('dve', s[0:rows, :], pv, ALU.add)
                P.act(sqv[0:rows, :], ps, AF.Square)
                P.red('dve', s2[0:rows, :], sqv[0:rows, :].rearrange("p (g c) -> p g c", g=4), ALU.add)
                P.ts('dve', s[0:rows, :], s[0:rows, :], 1.0 / 128, None, op0=ALU.mult)
                P.tt('dve', s3[0:rows, :], s[0:rows, :], s[0:rows, :], ALU.mult)
                P.stt('dve', s2[0:rows, :], s2[0:rows, :], 1.0 / 128, s3[0:rows, :], ALU.mult, ALU.subtract)
                P.act(s3[0:rows, :], s2[0:rows, :], AF.Sqrt, bias=EPS, scale=1.0)
                P.recip(s4[0:rows, :], s3[0:rows, :])
                P.stt('dve', s3[0:rows, :], s[0:rows, :], -1.0, s4[0:rows, :], ALU.mult, ALU.mult)
                for g in range(4):
                    P.ts('dve', gtmp[0:rows, g * 128:(g + 1) * 128], ps[:, g * 128:(g + 1) * 128],
                         s4[0:rows, g:g + 1], s3[0:rows, g:g + 1], op0=ALU.mult, op1=ALU.add)
                P.tt('dve', gtmp[0:rows, :], gtmp[0:rows, :], gsgB[0:rows, :], ALU.mult)
                P.tt('dve', out_f32, gtmp[0:rows, :], bsgB[0:rows, :], ALU.add)
                P.copy('dve', out_bf, out_f32)

            for g in range(int(os.environ.get('MK_NG', '8'))):
                for j in range(4):
                    blk = g * 4 + j
                    xb = xt[blk % 2]
                    P.dma('sp', xb[:], xp[blk * 128:(blk + 1) * 128, :])
                    norm_rows(xb[:], 128, xn[j][:])
                _chk(1)
                for dkp in range(4):
                    bk = nextbank()
                    pT = bk[:].bitcast(BF16)
                    for d2 in range(2):
                        dk = dkp * 2 + d2
                        for j in range(4):
                            P.tr(pT[:, d2 * 512 + j * 128: d2 * 512 + (j + 1) * 128], xn[j][:, dk * 128:(dk + 1) * 128], idb[:])
                    for d2 in range(2):
                        dk = dkp * 2 + d2
                        affine(evac_eng(), hT[:, dk, :], pT[:, d2 * 512:(d2 + 1) * 512], modT[:, 8 + dk, 0:1], modT[:, dk, 0:1])
                _chk(2)
                for j in range(4):
                    pblk = g * 4 + j
                    own = j < 2
                    orow = (2 * g + j) * 128
                    bk = nextbank()
                    for dk in range(8):
                        P.mm(bk[:], hT[:, dk, j * 128:(j + 1) * 128], WIN[:, dk, 512:1024], start=(dk == 0), stop=(dk == 7))
                    if own:
                        P.act(kst, bk[:], AF.Copy)
                        P.dma('sp', kp[orow:orow + 128, :], kst)
                    P.copy('dve', k16, bk[:])
                    bk2 = nextbank()
                    pKT = bk2[:].bitcast(BF16)
                    for h in range(4):
                        P.tr(pKT[:, h * 128:(h + 1) * 128], k16[:, h * 128:(h + 1) * 128], idb[:])
                    pcopy(evac_eng(), KT[:, :, pblk * 128:(pblk + 1) * 128], pKT[:, 0:512].rearrange("p (h t) -> p h t", h=4))
                    bk = nextbank()
                    for dk in range(8):
                        P.mm(bk[:], hT[:, dk, j * 128:(j + 1) * 128], WIN[:, dk, 1024:1536], start=(dk == 0), stop=(dk == 7))
                    if own:
                        P.act(vst, bk[:], AF.Copy)
                        P.dma('sp', vp[orow:orow + 128, :], vst)
                    P.copy('dve', VS[:, pblk, :], bk[:])
                _chk(3)
                t0 = 2 * g * 128
                for hh in range(2):
                    bk = nextbank()
                    for h2 in range(2):
                        h = hh * 2 + h2
                        for dk in range(8):
                            P.mm(bk[:, h2 * 256:(h2 + 1) * 256], WIN[:, dk, h * 128:(h + 1) * 128], hT[:, dk, 0:256],
                                 start=(dk == 0), stop=(dk == 7))
                    pcopy(evac_eng(), QT[:, hh * 2:hh * 2 + 2, t0:t0 + 256], bk[:].rearrange("p (h t) -> p h t", h=2))
                for hh in range(2):
                    bk = nextbank()
                    for h2 in range(2):
                        gi = hh * 2 + h2
                        for dk in range(8):
                            P.mm(bk[:, h2 * 256:(h2 + 1) * 256], WIN[:, dk, 1536 + gi * 128:1536 + (gi + 1) * 128], hT[:, dk, 0:256],
                                 start=(dk == 0), stop=(dk == 7))
                    pcopy(evac_eng(), uT[:, hh * 2:hh * 2 + 2, :], bk[:].rearrange("p (h t) -> p h t", h=2))
                for j in range(2):
                    bk = nextbank()
                    for dk in range(8):
                        P.mm(bk[:], hT[:, dk, j * 128:(j + 1) * 128], WIN[:, dk, 2048:2560], start=(dk == 0), stop=(dk == 7))
                    ln_groups(bk[:], 128, vln, vlb)
                    bk2 = nextbank()
                    for gi in range(4):
                        P.mm(bk2[:, gi * 128:(gi + 1) * 128], vlb[:, gi * 128:(gi + 1) * 128], wts[:, gi, :])
                    P.tt('dve', gtmp, bk2[:], bsB, ALU.add)
                    tt0 = t0 + j * 128
                    P.tt('dve', SGT[:, :, tt0:tt0 + 128], gtmp.rearrange("p (g t) -> p g t", g=4),
                         uT[:, :, j * 128:(j + 1) * 128], ALU.mult)

            if os.environ.get('MK_NOS'):
                raise _Stop()
            hTs = hT[:, :, 0:32]
            xsb = xt[0][0:32, :]
            P.dma('sp', xsb, xs)
            norm_rows(xsb, 32, xn[0][0:32, :])
            bk = nextbank()
            pT = bk[:].bitcast(BF16)
            for dk in range(8):
                P.tr(pT[:, dk * 32:(dk + 1) * 32], xn[0][0:32, dk * 128:(dk + 1) * 128], idb[0:32, 0:32])
            for dk in range(8):
                for s in range(4):
                    affine('dve', hT[:, dk, s * 8:(s + 1) * 8], pT[:, dk * 32 + s * 8: dk * 32 + (s + 1) * 8],
                           modT[:, 8 + dk, 1 + s:2 + s], modT[:, dk, 1 + s:2 + s])
            SK = sb("SK", [128, 4, 32], BF16)
            SVs = [xn[1 + s_ // 2][0:8, (s_ % 2) * 512:(s_ % 2 + 1) * 512] for s_ in range(4)]
            SQ = sb("SQ", [128, 4, 32], BF16)
            SGS = sb("SGS", [128, 4, 32], BF16)
            bk = nextbank()
            for dk in range(8):
                P.mm(bk[0:32, :], hT[:, dk, 0:32], WIN[:, dk, 512:1024], start=(dk == 0), stop=(dk == 7))
            P.act(kst[0:32, :], bk[0:32, :], AF.Copy)
            P.dma('sp', ks, kst[0:32, :])
            P.copy('dve', k16[0:32, :], bk[0:32, :])
            bk2 = nextbank()
            pKT = bk2[:].bitcast(BF16)
            for h in range(4):
                P.tr(pKT[:, h * 32:(h + 1) * 32], k16[0:32, h * 128:(h + 1) * 128], idb[0:32, 0:32])
            P.copy('dve', SK[:], pKT[:, 0:128].rearrange("p (h t) -> p h t", h=4))
            bk = nextbank()
            for dk in range(8):
                P.mm(bk[0:32, :], hT[:, dk, 0:32], WIN[:, dk, 1024:1536], start=(dk == 0), stop=(dk == 7))
            P.act(vst[0:32, :], bk[0:32, :], AF.Copy)
            P.dma('sp', vso, vst[0:32, :])
            for s_ in range(4):
                bk = nextbank()
                for dk in range(8):
                    P.mm(bk[0:8, :], hT[:, dk, s_ * 8:(s_ + 1) * 8], WIN[:, dk, 1024:1536], start=(dk == 0), stop=(dk == 7))
                P.copy('dve', SVs[s_], bk[0:8, :])
            bk = nextbank()
            for h in range(4):
                for dk in range(8):
                    P.mm(bk[:, h * 32:(h + 1) * 32], WIN[:, dk, h * 128:(h + 1) * 128], hT[:, dk, 0:32], start=(dk == 0), stop=(dk == 7))
            P.copy('dve', SQ[:], bk[:, 0:128].rearrange("p (h t) -> p h t", h=4))
            bk = nextbank()
            for gi in range(4):
                for dk in range(8):
                    P.mm(bk[:, gi * 32:(gi + 1) * 32], WIN[:, dk, 1536 + gi * 128:1536 + (gi + 1) * 128], hT[:, dk, 0:32],
                         start=(dk == 0), stop=(dk == 7))
            P.copy('dve', uT[:, :, 0:32], bk[:, 0:128].rearrange("p (h t) -> p h t", h=4))
            bk = nextbank()
            for dk in range(8):
                P.mm(bk[0:32, :], hT[:, dk, 0:32], WIN[:, dk, 2048:2560], start=(dk == 0), stop=(dk == 7))
            ln_groups(bk[0:32, :], 32, vln[0:32, :], vlb[0:32, :])
            P.dma('sp', sgv, vln[0:32, :])
            bk2 = nextbank()
            for gi in range(4):
                P.mm(bk2[:, gi * 32:(gi + 1) * 32], vlb[0:32, gi * 128:(gi + 1) * 128], wtsS[0:32, gi, :])
            P.tt('dve', gtmp[:, 0:128], bk2[:, 0:128], bsSB[:], ALU.add)
            P.tt('dve', SGS[:], gtmp[:, 0:128].rearrange("p (g t) -> p g t", g=4), uT[:, :, 0:32], ALU.mult)

            if STAGE <= 2:
                raise _Stop()
            P.memset('dve', sml[:, 20:22], 0.0)
            for (src, nch, col) in ((QT, 4, 20), (KT, 8, 21)):
                for h in range(4):
                    for c in range(nch):
                        P.act(PT[0], src[:, h, c * 512:(c + 1) * 512], AF.Square)
                        bk = nextbank()
                        P.mm(bk[:], onesb[:], PT[0])
                        s = sscols(4)
                        P.red('dve', s[:, 0:1], bk[:], ALU.max)
                        P.tt('dve', sml[:, col:col + 1], sml[:, col:col + 1], s[:, 0:1], ALU.max)
            P.tt('dve', sml[:, 22:23], sml[:, 20:21], sml[:, 21:22], ALU.add)
            P.ts('dve', sml[:, 23:24], sml[:, 22:23], -1.02 / 16.0, None, op0=ALU.mult)
            negC = sml[:, 23:24]

            PS = [pb[0], pb[1], pb[2], pb[3]]
            PO = [pb[4], pb[5]]
            PL = [pb[6], pb[7]]
            it = [0]
            for c in range(4):
                for h in range(4):
                    kbs = []
                    for u in range(4 * c + 4):
                        for pos in range(2):
                            r = max(u - 4 * c, 0)
                            sp_list = []
                            if pos == 0:
                                if u >= 4 * c:
                                    sp_list.append(((u - 4 * c) * 128, 0))
                                if u + 1 >= 4 * c and u + 1 < 4 * c + 4:
                                    sp_list.append(((u + 1 - 4 * c) * 128, 3 + (u + 1) % 2))
                            else:
                                if u >= 4 * c:
                                    sp_list.append(((u - 4 * c) * 128, 1 + u % 2))
                            kbs.append((_pblk(u, pos), r * 128, sp_list))
                    nk = len(kbs)
                    base_it = it[0]

                    def s_stage(ki):
                        pblk, col0, sp_list = kbs[ki]
                        par = (base_it + ki) % 2
                        for m in range(2):
                            ps_ = PS[par * 2 + m]
                            P.mm(ps_[:, col0:512], KT[m * 64:(m + 1) * 64, h, pblk * 128:(pblk + 1) * 128],
                                 QT[m * 64:(m + 1) * 64, h, c * 512 + col0:(c + 1) * 512])
                        for m in range(2):
                            ps_ = PS[par * 2 + m]
                            pt_ = PT[par * 2 + m]
                            P.act(pt_[:, col0:512], ps_[:, col0:512], AF.Exp, bias=negC, scale=0.125)
                            for (a, idx) in sp_list:
                                P.tt('dve', pt_[:, a:a + 128], pt_[:, a:a + 128], EB[:, idx, h, :], ALU.mult)

                    def av_stage(ki):
                        pblk, col0, sp_list = kbs[ki]
                        par = (base_it + ki) % 2
                        for m in range(2):
                            pt_ = PT[par * 2 + m]
                            P.mm(PO[m][:, col0:512], VS[:, pblk, h * 128:(h + 1) * 128], pt_[:, col0:512],
                                 start=(ki == 0), stop=(ki == nk - 1))
                            P.mm(PL[m][:, col0:512], onesb[:], pt_[:, col0:512], start=(ki == 0), stop=(ki == nk - 1))

                    s_stage(0)
                    for ki in range(1, nk):
                        s_stage(ki)
                        av_stage(ki - 1)
                    av_stage(nk - 1)
                    it[0] += nk
                    for m, on in ((0, ep_on0), (1, ep_on1)):
                        P.recip(ep_rl, PL[m][:])
                        P.tt('dve', on, PO[m][:], ep_rl, ALU.mult)
                    P.stt('dve', ep_o, ep_on1, neglam, ep_on0, ALU.mult, ALU.add)
                    P.act(ep_sq, ep_o, AF.Square)
                    bx = PS[0]
                    P.mm(bx[:], onesb[:], ep_sq)
                    P.act(ep_r, bx[:], AF.Sqrt, bias=EPS, scale=1.0 / 128)
                    P.recip(ep_rl, ep_r)
                    P.stt('dve', QT[:, h, c * 512:(c + 1) * 512], ep_o, gsubc, ep_rl, ALU.mult, ALU.mult)
            AOT = QT

            if STAGE <= 3:
                raise _Stop()
            for k in range(8):
                P.dma('pool', WO[:, k, :], w_o[k * 128:(k + 1) * 128, :])

            def b2_block(blk, rows, mT_chunks, x_src_ap, Gt, G2unused=None):
                xb = xt[blk % 2]
                P.dma('sp', xb[0:rows, :], x_src_ap)
                for half in range(2):
                    bk = nextbank()
                    for fc in range(8):
                        P.mm(bk[0:rows, :], mT_chunks[fc], WO[:, fc, half * 512:(half + 1) * 512], start=(fc == 0), stop=(fc == 7))
                    P.tt('dve', tmpf[0:rows, half * 512:(half + 1) * 512], bk[0:rows, :], Gt[0:rows, half * 512:(half + 1) * 512], ALU.mult)
                P.tt('dve', x1b[0:rows, :], tmpf[0:rows, :], xb[0:rows, :], ALU.add)
                P.dma('sp', x1scr[blk * 128:blk * 128 + rows, :], x1b[0:rows, :])
                norm_rows(x1b[0:rows, :], rows, xnf[0:rows, :])
                for hf in range(2):
                    bk = nextbank()
                    for d4 in range(4):
                        dk = hf * 4 + d4
                        P.tr(bk[:, d4 * 128:d4 * 128 + rows], xnf[0:rows, dk * 128:(dk + 1) * 128], idf[0:rows, 0:rows])
                    for d4 in range(4):
                        dk = hf * 4 + d4
                        if rows == 128:
                            affine(evac_eng(), h2Tf[:, dk, :], bk[:, d4 * 128:(d4 + 1) * 128], modT[:, 32 + dk, 0:1], modT[:, 24 + dk, 0:1])
                        else:
                            for s in range(4):
                                affine('dve', h2Tf[:, dk, s * 8:(s + 1) * 8], bk[:, d4 * 128 + s * 8:d4 * 128 + (s + 1) * 8],
                                       modT[:, 32 + dk, 1 + s:2 + s], modT[:, 24 + dk, 1 + s:2 + s])
                P.copy('dve', H2T[:, :, blk * 128:blk * 128 + rows], h2Tf[:, :, 0:rows])
                bk = nextbank()
                for dk in range(8):
                    P.mm(bk[0:rows, 0:20], h2Tf[:, dk, 0:rows], wrt[:, dk, :], start=(dk == 0), stop=(dk == 7))
                P.tt('dve', LG[0:rows, blk, :], bk[0:rows, 0:20], brB[0:rows, :], ALU.add)

            for t in range(16):
                chunks = [AOT[:, h, t * 128:(t + 1) * 128] for h in range(4)] + [SGT[:, g, t * 128:(t + 1) * 128] for g in range(4)]
                prow = _pblk(t, 0) * 128
                b2_block(t, 128, chunks, xp[prow:prow + 128, :], G1p)

            if STAGE <= 4:
                raise _Stop()
            SAO = sb("SAO", [128, 4, 32], BF16)
            if do_sample:

                S_all = A1[:, 33792:33792 + 16400].bitcast(F32)
                Pm = A1[:, 0:8200]
                ktl = [A1[:, 8704 + i * 1024: 8704 + (i + 1) * 1024].bitcast(F32) for i in range(3)]
                vtl = [A1[:, 11776 + i * 1024: 11776 + (i + 1) * 1024].bitcast(F32) for i in range(3)]
                vbl = [A1[:, 14848 + i * 512: 14848 + (i + 1) * 512] for i in range(2)]
                KTp = [A2[:, 8192 + i * 512: 8192 + (i + 1) * 512].rearrange("p (h t) -> p h t", h=4) for i in range(2)]
                PTp = [A2[:, 9216 + i * 128: 9216 + (i + 1) * 128] for i in range(2)]
                idx = A2[:, 9472:9984].bitcast(I32)
                ptb = A2[:, 9984:10496].bitcast(I32)
                SBT = A2[:, 10496:10768].bitcast(F32)
                smk = A2[:, 10768:11040].bitcast(F32)
                dsl = A2[:, 11040:11168].bitcast(F32)
                Dm = A2[:, 11168:11232].bitcast(F32)
                On = A2[:, 11232:11488].bitcast(F32)
                osb = A2[:, 11488:11744].bitcast(F32)
                PTn = A2[:, 11744:11872]
                sm2 = A2[:, 11872:11936].bitcast(F32)
                QB = xn[3][:, 0:512].rearrange("p (s h c) -> p s h c", s=4, h=4)
                P.dma('sp', ptb, pt_in.to_broadcast([128, 256]))
                P.dma('sp', sm2[:, 0:1], pidx_in)
                P.dma('sp', sm2[:, 1:2], sb31_in)
                P.dma('sp', SBT, sbias_in)
                P.dma('sp', smk, smask_in)
                P.dma('sp', dsl, dsel_in)
                P.ts('dve', idx, ptb, 128.0, sm2[:, 0:1], op0=ALU.mult, op1=ALU.add)
                P.ts('dve', SBT, SBT, sm2[:, 1:2], None, op0=ALU.subtract)
                P.tt('dve', SBT, SBT, smk, ALU.add)
                P.stt('dve', Dm, dsl[:, 32:64], neglam, dsl[:, 0:32], ALU.mult, ALU.add)
                P.memset('dve', QB, 0.0)
                for h in range(4):
                    P.copy('dve', QB[0:64, :, h, 0:8], SQ[0:64, h, :].rearrange("p (s q) -> p s q", s=4))
                    P.copy('dve', QB[64:128, :, h, 8:16], SQ[64:128, h, :].rearrange("p (s q) -> p s q", s=4))
                nbanks[0] = 7
                bO = pb[7]

                def gather(dst, table, col):
                    off = bass.IndirectOffsetOnAxis(ap=idx[:, col:col + 1], axis=0)
                    P.custom('pool', 'sw',
                             lambda e: e.indirect_dma_start(out=dst, out_offset=None, in_=table, in_offset=off),
                             [idx[:, col:col + 1]], [dst])

                def tp(h):
                    return dict(tile_position=(0, 96)) if h == 3 else {}

                gi = [0]
                for s_ in range(4):
                    ktps = {}
                    bSs = {}

                    def k_stage(j):
                        kt = ktl[gi[0] % 3]
                        kb_ = vbl[gi[0] % 2]
                        ktp = KTp[gi[0] % 2]
                        gi[0] += 1
                        gather(kt, cache_k, s_ * 64 + j)
                        P.copy('dve', kb_, kt)
                        bkT = nextbank()
                        bkv = bkT[:].bitcast(BF16)
                        for h in range(4):
                            P.tr(bkv[:, h * 128:(h + 1) * 128], kb_[:, h * 128:(h + 1) * 128], idb[:])
                        P.act(ktp, bkv[:, 0:512].rearrange("p (h t) -> p h t", h=4), AF.Copy)
                        ktps[j] = ktp

                    def sc_stage(j):
                        ktp = ktps.pop(j)
                        if j % 4 == 0:
                            bSs[j // 4] = nextbank()
                        bS_ = bSs[j // 4]
                        for h in range(4):
                            P.mm(bS_[32 * h:32 * h + 32, (j % 4) * 128:(j % 4 + 1) * 128], QB[:, s_, h, :], ktp[:, h, :], **tp(h))
                        if j % 4 == 3:
                            P.act(S_all[:, (j - 3) * 128:(j + 1) * 128], bS_[:], AF.Copy, scale=0.125)
                            del bSs[j // 4]

                    k_stage(0)
                    for j in range(1, 64):
                        k_stage(j)
                        sc_stage(j - 1)
                    sc_stage(63)
                    bS = nextbank()
                    for h in range(4):
                        P.mm(bS[32 * h:32 * h + 32, 0:8], QB[:, s_, h, :], SK[:, h, s_ * 8:(s_ + 1) * 8], **tp(h))
                    P.act(S_all[:, 8192:8200], bS[:, 0:8], AF.Copy, scale=0.125)
                    P.tt('dve', S_all[:, 8064:8200], S_all[:, 8064:8200], SBT, ALU.add)
                    P.red('dve', sm2[:, 4:5], S_all, ALU.max)
                    P.ts('dve', sm2[:, 5:6], sm2[:, 4:5], -1.0, None, op0=ALU.mult)
                    P.act(Pm, S_all, AF.Exp, bias=sm2[:, 5:6], scale=1.0, accum_out=sm2[:, 6:7])
                    P.recip(sm2[:, 7:8], sm2[:, 6:7])
                    pv = {}

                    def p_stage(j):
                        vt = vtl[gi[0] % 3]
                        vb = vbl[gi[0] % 2]
                        ptp = PTp[gi[0] % 2]
                        gi[0] += 1
                        gather(vt, cache_v, s_ * 64 + j)
                        P.copy('dve', vb, vt)
                        bP = nextbank()
                        bPv = bP[:].bitcast(BF16)
                        P.tr(bPv[:, 0:128], Pm[:, j * 128:(j + 1) * 128], idb[:])
                        P.act(ptp, bPv[:, 0:128], AF.Copy)
                        pv[j] = (ptp, vb)

                    def av2_stage(j):
                        ptp, vb = pv.pop(j)
                        for h in range(4):
                            P.mm(bO[32 * h:32 * h + 32, 0:128], ptp[:, 32 * h:32 * h + 32], vb[:, h * 128:(h + 1) * 128],
                                 start=(j == 0), stop=False, **tp(h))

                    p_stage(0)
                    for j in range(1, 64):
                        p_stage(j)
                        av2_stage(j - 1)
                    av2_stage(63)
                    bP = nextbank()
                    bPv = bP[:].bitcast(BF16)
                    P.tr(bPv[0:8, 0:128], Pm[:, 8192:8200], idb[:])
                    P.act(PTn[0:8, :], bPv[0:8, 0:128], AF.Copy)
                    for h in range(4):
                        P.mm(bO[32 * h:32 * h + 32, 0:128], PTn[0:8, 32 * h:32 * h + 32], SVs[s_][:, h * 128:(h + 1) * 128],
                             start=False, stop=True, **tp(h))
                    P.ts('dve', On, bO[:, 0:128], sm2[:, 7:8], None, op0=ALU.mult)
                    bC = nextbank()
                    P.mm(bC[0:32, 0:128], Dm, On)
                    P.act(sq[0:32, 0:128], bC[0:32, 0:128], AF.Square, accum_out=sm2[0:32, 8:9])
                    P.act(sm2[0:32, 9:10], sm2[0:32, 8:9], AF.Sqrt, bias=EPS, scale=1.0 / 128)
                    P.recip(sm2[0:32, 10:11], sm2[0:32, 9:10])
                    P.ts('dve', osb[0:32, :], bC[0:32, 0:128], sm2[0:32, 10:11], None, op0=ALU.mult)
                    bT = nextbank()
                    P.tr(bT[:, 0:32], osb[0:32, :], idf[0:32, 0:32])
                    P.ts('dve', SAO[:, :, s_ * 8:(s_ + 1) * 8], bT[:, 0:32].rearrange("p (h q) -> p h q", h=4), gsubc, None, op0=ALU.mult)
                nbanks[0] = 8
            else:
                P.memset('dve', SAO[:], 0.0)
            chunks = [SAO[:, h, :] for h in range(4)] + [SGS[:, g, :] for g in range(4)]
            P.memset('dve', LG[:, 16, :], 0.0)
            b2_block(16, 32, chunks, xs, G1s)

            if STAGE <= 5:
                raise _Stop()
            router(P, LG, COMB, RT)
            slot = [0]

            def eload(src2d_rows, ncols, kparts):
                v = ESLOT[slot[0] % 4].rearrange("p (k n) -> p k n", k=kparts)
                slot[0] += 1
                for k in range(kparts):
                    P.dma('pool', v[:, k, :], src2d_rows(k))
                return v

            groups = [(0, 512, 4), (512, 512, 4), (1024, 512, 4), (1536, 512, 4), (2048, 32, 1)]
            hi = [0]
            for e in range(16):
                wg = eload(lambda k: w_gate[e, k * 128:(k + 1) * 128, :], 512, 8)
                wu = eload(lambda k: w_up[e, k * 128:(k + 1) * 128, :], 512, 8)
                wd = eload(lambda k: w_down[e, k * 128:(k + 1) * 128, :], 1024, 4)
                for (c0, N, nb) in groups:
                    hb = he[hi[0] % 2]
                    hi[0] += 1
                    for fc in range(4):
                        bg = nextbank()
                        bu = nextbank()
                        for dk in range(8):
                            P.mm(bg[:, 0:N], wg[:, dk, fc * 128:(fc + 1) * 128], H2T[:, dk, c0:c0 + N], start=(dk == 0), stop=(dk == 7))
                        for dk in range(8):
                            P.mm(bu[:, 0:N], wu[:, dk, fc * 128:(fc + 1) * 128], H2T[:, dk, c0:c0 + N], start=(dk == 0), stop=(dk == 7))
                        P.act(sil[:, 0:N], bg[:, 0:N], AF.Silu)
                        P.tt('dve', hb[:, fc, 0:N], sil[:, 0:N], bu[:, 0:N], ALU.mult)
                    for b in range(nb):
                        blk = c0 // 128 + b
                        rows = 128 if N == 512 else 32
                        for half in range(2):
                            by = nextbank()
                            for fc in range(4):
                                P.mm(by[0:rows, :], hb[:, fc, b * 128:b * 128 + rows], wd[:, fc, half * 512:(half + 1) * 512],
                                     start=(fc == 0), stop=(fc == 3))
                            ya = yacc(blk)[0:rows, half * 512:(half + 1) * 512]
                            if e == 0:
                                P.ts('dve', ya, by[0:rows, :], COMB[0:rows, blk, e:e + 1], None, op0=ALU.mult)
                            else:
                                P.stt('dve', ya, by[0:rows, :], COMB[0:rows, blk, e:e + 1], ya, ALU.mult, ALU.add)

            gfin = W3[:, 8192:10240].bitcast(F32)
            P.dma('sp', gfin, gfin_in.to_broadcast([128, D]))
            for blk in range(17):
                rows = 128 if blk < 16 else 32
                Gt = G2p if blk < 16 else G2s
                xb = xt[blk % 2]
                P.dma('sp', xb[0:rows, :], x1scr[blk * 128:blk * 128 + rows, :])
                P.tt('dve', tmpf[0:rows, :], yacc(blk)[0:rows, :], Gt[0:rows, :], ALU.mult)
                P.tt('dve', x1b[0:rows, :], tmpf[0:rows, :], xb[0:rows, :], ALU.add)
                norm_rows(x1b[0:rows, :], rows, xnf[0:rows, :])
                P.tt('dve', tmpf[0:rows, :], xnf[0:rows, :], gfin[0:rows, :], ALU.mult)
                if blk < 16:
                    P.dma('sp', yp[blk * 128:(blk + 1) * 128, :], tmpf[:, :])
                else:
                    P.dma('sp', ys, tmpf[0:32, :])


      except _Stop:
        pass
      P.emit()
    return nc, P


def router(P, LG, COMB, RT):
    lg = LG[:, :, 0:4]
    le = LG[:, :, 4:20].rearrange("p b (g j) -> p b g j", g=4)
    gmax = RT[:, :, 0:1]
    P.red('dve', RT[:, :, 0], lg, ALU.max)
    gsel = RT[:, :, 1:5]
    P.tt('dve', gsel, lg, gmax.to_broadcast([128, 17, 4]), ALU.is_equal)
    P.tt('dve', RT[:, :, 5:9], lg, gmax.to_broadcast([128, 17, 4]), ALU.subtract)
    P.act(RT[:, :, 5:9], RT[:, :, 5:9], AF.Exp)
    P.red('dve', RT[:, :, 9], RT[:, :, 5:9], ALU.add)
    P.recip(RT[:, :, 9], RT[:, :, 9])
    gp = RT[:, :, 9:10]
    sel = RT[:, :, 10:14]
    for g in range(4):
        if g == 0:
            P.tt('dve', sel, le[:, :, 0, :], gsel[:, :, 0:1].to_broadcast([128, 17, 4]), ALU.mult)
        else:
            P.tt('dve', RT[:, :, 5:9], le[:, :, g, :], gsel[:, :, g:g + 1].to_broadcast([128, 17, 4]), ALU.mult)
            P.tt('dve', sel, sel, RT[:, :, 5:9], ALU.add)
    m1 = RT[:, :, 14:15]
    P.red('dve', RT[:, :, 14], sel, ALU.max)
    mask1 = RT[:, :, 15:19]
    P.tt('dve', mask1, sel, m1.to_broadcast([128, 17, 4]), ALU.is_equal)
    sel2 = RT[:, :, 5:9]
    P.stt('dve', sel2, mask1, -1e30, sel, ALU.mult, ALU.add)
    m2 = RT[:, :, 19:20]
    P.red('dve', RT[:, :, 19], sel2, ALU.max)
    mask2 = RT[:, :, 20:24]
    P.tt('dve', mask2, sel2, m2.to_broadcast([128, 17, 4]), ALU.is_equal)
    d = RT[:, :, 0:1]
    P.tt('dve', d, m2, m1, ALU.subtract)
    P.act(d, d, AF.Exp)
    w1 = RT[:, :, 14:15]
    P.ts('dve', w1, d, 1.0, None, op0=ALU.add)
    P.recip(RT[:, :, 14], RT[:, :, 14])
    w2 = RT[:, :, 19:20]
    P.tt('dve', w2, d, w1, ALU.mult)
    P.tt('dve', w1, w1, gp, ALU.mult)
    P.tt('dve', w2, w2, gp, ALU.mult)
    cig = RT[:, :, 5:9]
    P.tt('dve', cig, mask1, w1.to_broadcast([128, 17, 4]), ALU.mult)
    P.tt('dve', mask2, mask2, w2.to_broadcast([128, 17, 4]), ALU.mult)
    P.tt('dve', cig, cig, mask2, ALU.add)
    cv = COMB.rearrange("p b (g j) -> p b g j", g=4) if False else None
    for g in range(4):
        P.tt('dve', COMB[:, :, g * 4:(g + 1) * 4], cig, gsel[:, :, g:g + 1].to_broadcast([128, 17, 4]), ALU.mult)


def sample_attention(P, nc, L):
    raise NotImplementedError


def _t5_bucket_np(n):
    n = np.maximum(n, 0)
    nf = np.maximum(n, 1).astype(np.float32)
    large = 16 + (np.log(nf / 16) / np.log(128 / 16) * 16).astype(np.int32)
    large = np.minimum(large, 31)
    return np.where(n < 16, n, large)


_CACHE = {}
DO_SAMPLE = True


def kernel(x_prompt, x_sample, c_prompt, c_sample, cache_k, cache_v, page_table,
           w_ada, b_ada, w_in, w_o, lam_q1, lam_k1, lam_q2, lam_k2, g_subln, rel_bias,
           g_sg_ln, b_sg_ln, w_s, b_s, w_rg, b_rg, w_re, b_re, w_gate, w_up, w_down, g_final):
    f32 = np.float32
    A = lambda a: np.ascontiguousarray(np.asarray(a))
    x_prompt = A(x_prompt); x_sample = A(x_sample)
    rel_bias = A(rel_bias)
    key = ('nc', DO_SAMPLE)
    if key not in _CACHE:
        _CACHE[key] = build_program(DO_SAMPLE)
    nc, P = _CACHE[key]

    kk = np.arange(128)[:, None]
    qq = np.arange(128)[None, :]
    dist_diag = qq - kk
    dist_sub = 128 + qq - kk
    dist_far = np.full((128, 128), 100000)
    bk_diag = _t5_bucket_np(dist_diag)
    bk_sub = _t5_bucket_np(dist_sub)
    bk_far = _t5_bucket_np(dist_far)
    m_diag = (dist_diag >= 0).astype(f32)
    m_one = np.ones((128, 128), f32)
    m_zero = np.zeros((128, 128), f32)
    tril = (np.arange(128)[:, None] <= np.arange(128)[None, :]).astype(f32)
    ii = np.arange(32)
    trilS = ((ii[:, None] // 8 == ii[None, :] // 8) & (ii[:, None] <= ii[None, :])).astype(f32)
    sel = np.zeros((5, 160), f32)
    sel[0, 0:128] = 1.0
    for s in range(4):
        sel[1 + s, 128 + s * 8:128 + (s + 1) * 8] = 1.0
    ident = np.eye(128).astype(ml_dtypes.bfloat16)
    identf = np.eye(128, dtype=f32)

    w_s0 = A(w_s)[0]
    wsT = np.ascontiguousarray(np.transpose(w_s0, (2, 0, 1)).reshape(128, 512))
    wsS = np.zeros((32, 4, 32), f32)
    for s in range(4):
        wsS[s * 8:(s + 1) * 8, :, s * 8:(s + 1) * 8] = np.transpose(w_s0[:, :8, :8], (2, 0, 1))
    wsS = wsS.reshape(32, 128)
    bs0 = A(b_s)[0]
    bsS = np.ascontiguousarray(bs0[:, np.arange(32) % 8]).reshape(1, 128)
    wr = np.ascontiguousarray(np.concatenate([A(w_rg)[0], A(w_re)[0]], axis=1))
    br = np.ascontiguousarray(np.concatenate([A(b_rg)[0], A(b_re)[0]])[None])
    lam4 = np.ascontiguousarray(np.concatenate([A(lam_q1)[0], A(lam_k1)[0], A(lam_q2)[0], A(lam_k2)[0]])[None])

    shared = dict(
        w_ada=A(w_ada)[0], b_ada=A(b_ada), w_in=A(w_in)[0], w_o=A(w_o)[0], lam4=lam4,
        gsub=np.ascontiguousarray(A(g_subln).reshape(128, 1)),
        b31=np.ascontiguousarray(rel_bias[31:32, :]),
        gsg=A(g_sg_ln).reshape(1, 512), bsg=A(b_sg_ln).reshape(1, 512),
        wsT=wsT, tril=tril, wsS=wsS, trilS=trilS, bs=bs0.reshape(1, 512), bsS=bsS,
        wr=wr, br=br, w_gate=A(w_gate)[0], w_up=A(w_up)[0], w_down=A(w_down)[0],
        gfin=A(g_final).reshape(1, D), ident=ident, identf=identf, sel=sel,
    )
    if DO_SAMPLE:
        shared['cache_k'] = A(cache_k).reshape(2560 * 128, 512)
        shared['cache_v'] = A(cache_v).reshape(2560 * 128, 512)
        pp = np.arange(128)
        hh = pp // 32
        q_of = (pp % 32) % 8
        cc = np.arange(136)
        dist_s = np.where(cc[None, :] < 128, 128 + q_of[:, None] - cc[None, :], q_of[:, None] - (cc[None, :] - 128))
        shared['sbias'] = np.ascontiguousarray(rel_bias[_t5_bucket_np(dist_s), hh[:, None]]).astype(f32)
        shared['smask'] = np.where(dist_s >= 0, 0.0, NEG).astype(f32)
        shared['sb31'] = np.ascontiguousarray(rel_bias[31, hh][:, None]).astype(f32)
        dsel = np.zeros((128, 64), f32)
        for h in range(4):
            for q in range(8):
                dsel[h * 32 + q, h * 8 + q] = 1.0
                dsel[h * 32 + 8 + q, 32 + h * 8 + q] = 1.0
        shared['dsel'] = dsel
        shared['pidx'] = np.arange(128, dtype=f32).reshape(128, 1)

    in_maps = []
    for c in range(NCORES):
        b, cpar = c // 2, c % 2
        order = []
        for g in range(8):
            order += [_own_block(cpar, 2 * g), _own_block(cpar, 2 * g + 1),
                      _partner_block(cpar, 2 * g), _partner_block(cpar, 2 * g + 1)]
        xb = x_prompt[b].reshape(32, 128, D)
        xpc = np.ascontiguousarray(xb[order].reshape(4096, D))
        cinc = np.ascontiguousarray(np.concatenate([A(c_prompt)[b:b + 1], A(c_sample)[4 * c:4 * c + 4]], axis=0))
        bks, mks = [bk_diag], [m_diag]
        for tau in (0, 1):
            second = (tau + cpar) % 2 == 1
            bks.append(bk_sub if second else bk_far)
            mks.append(m_one if second else m_zero)
        for tau in (0, 1):
            second = (tau + cpar) % 2 == 1
            bks.append(bk_far if second else bk_sub)
            mks.append(m_one)
        bg = np.stack([rel_bias[bk] for bk in bks], axis=0)
        bg = np.ascontiguousarray(np.transpose(bg, (1, 0, 3, 2)).reshape(128, 5 * 4 * 128)).astype(f32)
        mg = np.ascontiguousarray(np.transpose(np.stack(mks, 0), (1, 0, 2)).reshape(128, 5 * 128)).astype(f32)
        m = dict(shared)
        m.update(xp=xpc, xs=np.ascontiguousarray(x_sample[4 * c:4 * c + 4].reshape(32, D)), cin=cinc, biasg=bg, maskg=mg)
        if DO_SAMPLE:
            m['pt'] = np.ascontiguousarray(A(page_table)[4 * c:4 * c + 4].reshape(1, 256)).astype(np.int32)
        in_maps.append(m)

    ncr = int(os.environ.get('MK_CORES', str(NCORES)))
    res = run_bass_kernel_spmd(nc, in_maps[:ncr], core_ids=list(range(ncr)))
    R = list(res.results) + [res.results[0]] * (NCORES - ncr)
    y_prompt = np.zeros((4, 4096, D), f32)
    kp = np.zeros((4, 1, 4096, 4, 128), f32)
    vp = np.zeros((4, 1, 4096, 4, 128), f32)
    y_sample = np.zeros((32, 8, D), f32)
    ks = np.zeros((32, 1, 8, 4, 128), f32)
    vs = np.zeros((32, 1, 8, 4, 128), f32)
    sg = np.zeros((32, 1, 8, 4, 128), f32)
    for c in range(NCORES):
        b, cpar = c // 2, c % 2
        r = R[c]
        for t in range(16):
            gblk = _own_block(cpar, t)
            y_prompt[b, gblk * 128:(gblk + 1) * 128] = r['yp'][t * 128:(t + 1) * 128]
            kp[b, 0, gblk * 128:(gblk + 1) * 128] = r['kp'][t * 128:(t + 1) * 128].reshape(128, 4, 128)
            vp[b, 0, gblk * 128:(gblk + 1) * 128] = r['vp'][t * 128:(t + 1) * 128].reshape(128, 4, 128)
        y_sample[4 * c:4 * c + 4] = r['ys'].reshape(4, 8, D)
        ks[4 * c:4 * c + 4, 0] = r['ks'].reshape(4, 8, 4, 128)
        vs[4 * c:4 * c + 4, 0] = r['vso'].reshape(4, 8, 4, 128)
        sg[4 * c:4 * c + 4, 0] = r['sgv'].reshape(4, 8, 4, 128)
    return (y_prompt, y_sample, kp, vp, ks, vs, sg)
```

```python
import numpy as np
import concourse.bass as bass
import concourse.mybir as mybir

F32 = mybir.dt.float32
BF16 = mybir.dt.bfloat16
I32 = mybir.dt.int32
AF = mybir.ActivationFunctionType
ALU = mybir.AluOpType
AX = mybir.AxisListType

_ESZ = {}


def _esz(dt):
    s = str(dt)
    if s not in _ESZ:
        if '32' in s:
            _ESZ[s] = 4
        elif '16' in s:
            _ESZ[s] = 2
        elif '64' in s:
            _ESZ[s] = 8
        else:
            _ESZ[s] = 1
    return _ESZ[s]


def box(ap):
    t = ap.tensor
    name = t.name
    esz = _esz(ap.dtype)
    dims = ap.ap
    off = ap.offset
    if not isinstance(off, int):
        return (name, 0, 1 << 30, 0, 1 << 40)
    tn = type(t).__name__
    if tn.startswith('PSum'):
        return ('PSUM:' + name, 0, 128, 0, 1 << 20)
    if tn.startswith('SB'):
        pstep, pcnt = dims[0]
        if pstep == 0:
            pstep = 1 << 40
        p0 = off // pstep + getattr(t, 'base_partition', 0) if pstep < (1 << 40) else 0
        f0 = off % pstep if pstep < (1 << 40) else off
        ext = sum((c - 1) * abs(s) for s, c in dims[1:])
        return (name, p0, p0 + pcnt, f0 * esz, (f0 + ext + 1) * esz)
    ext = sum((c - 1) * abs(s) for s, c in dims)
    return (name, 0, 1, off * esz, (off + ext + 1) * esz)


class Op:
    __slots__ = ('id', 'eng', 'kind', 'fn', 'deps', 'marked', 'rank', 'sem', 'semval', 'waits')

    def __init__(self, id, eng, kind, fn):
        self.id = id
        self.eng = eng
        self.kind = kind
        self.fn = fn
        self.deps = {}
        self.marked = False
        self.rank = 0
        self.sem = None
        self.semval = 0
        self.waits = []


EPOCH = 2000


class Prog:
    def __init__(self, nc, n_dma_sems=40):
        self.nc = nc
        self.ops = []
        self.hist = {}
        self.n_dma_sems = n_dma_sems
        self.track_off = set()

    def _add(self, eng, kind, fn, reads, writes):
        op = Op(len(self.ops), eng, kind, fn)
        self.ops.append(op)
        for ap in reads:
            if ap is None or isinstance(ap, (int, float)):
                continue
            self._access(op, box(ap), False)
        for ap in writes:
            if ap is None:
                continue
            self._access(op, box(ap), True)
        return op

    def _access(self, op, b, is_write):
        name, plo, phi, flo, fhi = b
        if name in self.track_off:
            return
        h = self.hist.setdefault(name, [])
        keep = []
        for e in h:
            eplo, ephi, eflo, efhi, eid, ew = e
            if eid == op.id:
                keep.append(e)
                continue
            ov = not (ephi <= plo or phi <= eplo or efhi <= flo or fhi <= eflo)
            if ov and name.startswith('PSUM:') and not ew and not is_write and self.ops[eid].eng != op.eng:
                op.deps.setdefault(eid, 'rar')
            if ov:
                if ew or is_write:
                    typ = 'raw' if (ew and not is_write) else ('waw' if ew else 'war')
                    old = op.deps.get(eid)
                    if old is None or typ == 'raw':
                        op.deps[eid] = typ
                if is_write and plo <= eplo and ephi <= phi and flo <= eflo and efhi <= fhi:
                    continue
            keep.append(e)
        if not is_write:
            if op.kind == 'c':
                keep2 = []
                for e in keep:
                    if (not e[5]) and e[0] == plo and e[1] == phi and e[2] == flo and e[3] == fhi:
                        o = self.ops[e[4]]
                        if o.kind == 'c' and o.eng == op.eng:
                            continue
                    keep2.append(e)
                keep = keep2
        keep.append((plo, phi, flo, fhi, op.id, is_write))
        self.hist[name] = keep

    def mm(self, out, lhsT, rhs, start=True, stop=True, **kw):
        return self._add('pe', 'c', lambda e: e.matmul(out, lhsT, rhs, start=start, stop=stop, **kw),
                         [lhsT, rhs], [out])

    def tr(self, out, in_, ident):
        return self._add('pe', 'c', lambda e: e.transpose(out, in_, ident), [in_, ident], [out])

    def act(self, out, in_, func, bias=0.0, scale=1.0, accum_out=None):
        rd = [in_]
        if not isinstance(bias, (int, float)):
            rd.append(bias)
        if not isinstance(scale, (int, float)):
            rd.append(scale)
        kw = {}
        if accum_out is not None:
            kw['accum_out'] = accum_out
        return self._add('act', 'c', lambda e: e.activation(out, in_, func, bias=bias, scale=scale, **kw),
                         rd, [out, accum_out])

    def ts(self, eng, out, in0, s1, s2=None, op0=ALU.mult, op1=None, accum_out=None):
        rd = [in0]
        if not isinstance(s1, (int, float)):
            rd.append(s1)
        if s2 is not None and not isinstance(s2, (int, float)):
            rd.append(s2)
        kw = {}
        if op1 is not None:
            kw['op1'] = op1
        if accum_out is not None:
            kw['accum_out'] = accum_out
        return self._add(eng, 'c', lambda e: e.tensor_scalar(out, in0, s1, s2, op0, **kw), rd, [out, accum_out])

    def tt(self, eng, out, in0, in1, op):
        return self._add(eng, 'c', lambda e: e.tensor_tensor(out, in0, in1, op), [in0, in1], [out])

    def stt(self, eng, out, in0, scalar, in1, op0, op1):
        rd = [in0, in1]
        if not isinstance(scalar, (int, float)):
            rd.append(scalar)
        return self._add(eng, 'c', lambda e: e.scalar_tensor_tensor(out, in0, scalar, in1, op0, op1), rd, [out])

    def copy(self, eng, out, in_):
        if eng == 'act':
            return self._add('act', 'c', lambda e: e.copy(out, in_), [in_], [out])
        return self._add(eng, 'c', lambda e: e.tensor_copy(out, in_), [in_], [out])

    def red(self, eng, out, in_, op, axis=AX.X):
        return self._add(eng, 'c', lambda e: e.tensor_reduce(out, in_, axis, op), [in_], [out])

    def recip(self, out, in_):
        return self._add('dve', 'c', lambda e: e.reciprocal(out, in_), [in_], [out])

    def memset(self, eng, ap, val):
        return self._add(eng, 'c', lambda e: e.memset(ap, val), [], [ap])

    def dma(self, q, out, in_, extra_reads=(), **kw):
        kind = 'sw' if q == 'pool' else 'dma'
        return self._add(q, kind, lambda e: e.dma_start(out=out, in_=in_, **kw), [in_] + list(extra_reads), [out])

    def custom(self, eng, kind, fn, reads, writes):
        return self._add(eng, kind, fn, reads, writes)

    def emit(self):
        nc = self.nc
        ops = self.ops
        n_hw = self.n_dma_sems
        n_sw = 8
        last_on_sem = {}
        di = {'dma': 0, 'sw': 0}
        for op in ops:
            if op.kind in ('dma', 'sw'):
                if op.kind == 'dma':
                    s = di['dma'] % n_hw
                else:
                    s = n_hw + di['sw'] % n_sw
                di[op.kind] += 1
                op.sem = s
                prev = last_on_sem.get(s)
                op.semval = (prev.semval if prev else 0) + 16
                if prev is not None:
                    op.deps.setdefault(prev.id, 'waw')
                last_on_sem[s] = op

        def skip(p, op, typ):
            if p.kind != 'c':
                return False
            if p.eng == 'pe' and op.eng == 'pe' and op.kind == 'c':
                return True
            return False

        for op in ops:
            for d, typ in op.deps.items():
                p = ops[d]
                if p.kind == 'c' and not skip(p, op, typ):
                    p.marked = True
        cnt = {}
        for op in ops:
            if op.kind == 'c' and op.marked:
                cnt[op.eng] = cnt.get(op.eng, 0) + 1
                op.rank = cnt[op.eng]
        engs = ['pe', 'act', 'dve', 'pool', 'sp']
        waited = {e: {} for e in engs}
        nwaits = 0
        for op in ops:
            need = {}
            for d, typ in op.deps.items():
                p = ops[d]
                if p.kind == 'c':
                    if skip(p, op, typ):
                        continue
                    key = ('e', p.eng, (p.rank - 1) // EPOCH)
                    val = (p.rank - 1) % EPOCH + 1
                else:
                    key = ('d', p.sem)
                    val = p.semval
                if need.get(key, 0) < val:
                    need[key] = val
            w = waited[op.eng]
            for key, val in need.items():
                if w.get(key, 0) >= val:
                    continue
                w[key] = val
                op.waits.append((key, val))
                nwaits += 1
        self.stats = dict(n_ops=len(ops), n_waits=nwaits, marked=dict(cnt), n_dma=dict(di))
        import contextlib
        with contextlib.ExitStack() as st:
            esem = {}
            for e in ['pe', 'act', 'dve', 'pool']:
                for k in range((cnt.get(e, 0) + EPOCH - 1) // EPOCH + 1):
                    esem[(e, k)] = st.enter_context(nc.semaphore('es_%s_%d' % (e, k)))
            dsem = [st.enter_context(nc.semaphore('ds_%d' % i)) for i in range(n_hw + n_sw)]
            block = st.enter_context(nc.Block())
            per = {e: [o for o in ops if o.eng == e] for e in engs}

            def run(e, eng):
                for op in per[e]:
                    for key, val in op.waits:
                        sem = esem[(key[1], key[2])] if key[0] == 'e' else dsem[key[1]]
                        eng.wait_ge(sem, val)
                    ins = op.fn(eng)
                    if op.kind in ('dma', 'sw'):
                        ins.then_inc(dsem[op.sem], 16)
                    elif op.marked:
                        ins.then_inc(esem[(e, (op.rank - 1) // EPOCH)], 1)
                if e == 'sp':
                    for s_, o in last_on_sem.items():
                        eng.wait_ge(dsem[s_], o.semval)

            @block.tensor
            def _(eng):
                run('pe', eng)

            @block.scalar
            def _(eng):
                run('act', eng)

            @block.vector
            def _(eng):
                run('dve', eng)

            @block.gpsimd
            def _(eng):
                run('pool', eng)

            @block.sync
            def _(eng):
                run('sp', eng)

import contextlib
import ml_dtypes
from concourse.bass_utils import run_bass_kernel_spmd

D = 1024
EPS = 1e-6
LAM_INIT = 0.2
NCORES = 8
NEG = -30000.0


def _own_block(cpar, t):
    return 2 * t + ((t + cpar) % 2)


def _partner_block(cpar, t):
    return 2 * t + 1 - ((t + cpar) % 2)


def _pblk(t, pos):
    return (t // 2) * 4 + (t % 2) + 2 * pos


import os
STAGE = int(os.environ.get('MK_STAGE', '9'))
SUB = int(os.environ.get('MK_SUB', '99'))


def _chk(n):
    if SUB <= n:
        raise _Stop()


class _Stop(Exception):
    pass


def build_program(do_sample=True):
    nc = bass.Bass("TRN2", target_bir_lowering=False)

    def din(name, shape, dt=F32):
        return nc.dram_tensor(name, list(shape), dt, kind="ExternalInput").ap()

    def dout(name, shape, dt=F32):
        return nc.dram_tensor(name, list(shape), dt, kind="ExternalOutput").ap()

    xp = din("xp", [4096, D])
    xs = din("xs", [32, D])
    cin = din("cin", [5, D])
    w_ada = din("w_ada", [D, 6 * D])
    b_ada = din("b_ada", [1, 6 * D])
    w_in = din("w_in", [D, 2560])
    w_o = din("w_o", [D, D])
    lam4 = din("lam4", [1, 256])
    gsub_in = din("gsub", [128, 1])
    biasg = din("biasg", [128, 5 * 4 * 128])
    maskg = din("maskg", [128, 5 * 128])
    b31_in = din("b31", [1, 4])
    gsg_in = din("gsg", [1, 512])
    bsg_in = din("bsg", [1, 512])
    wsT_in = din("wsT", [128, 4 * 128])
    tril_in = din("tril", [128, 128])
    wsS_in = din("wsS", [32, 4 * 32])
    trilS_in = din("trilS", [32, 32])
    bs_in = din("bs", [1, 512])
    bsS_in = din("bsS", [1, 128])
    wr_in = din("wr", [D, 20])
    br_in = din("br", [1, 20])
    w_gate = din("w_gate", [16, D, 512])
    w_up = din("w_up", [16, D, 512])
    w_down = din("w_down", [16, 512, D])
    gfin_in = din("gfin", [1, D])
    ident_in = din("ident", [128, 128], BF16)
    identf_in = din("identf", [128, 128])
    sel_in = din("sel", [5, 160])
    if do_sample:
        cache_k = din("cache_k", [2560 * 128, 512])
        cache_v = din("cache_v", [2560 * 128, 512])
        pt_in = din("pt", [128, 64], I32)
        sbias_in = din("sbias", [128, 136])
        smask_in = din("smask", [128, 136])
        sb31_in = din("sb31", [128, 1])
        dsel_in = din("dsel", [128, 64])
        pidx_in = din("pidx", [128, 1])

    yp = dout("yp", [2048, D])
    ys = dout("ys", [32, D])
    kp = dout("kp", [2048, 512])
    vp = dout("vp", [2048, 512])
    ks = dout("ks", [32, 512])
    vso = dout("vso", [32, 512])
    sgv = dout("sgv", [32, 512])
    x1scr = nc.dram_tensor("x1scr", [2176, D], F32, kind="Internal").ap()

    P = Prog(nc)
    with contextlib.ExitStack() as st:
      try:
            def sb(name, shape, dt):
                return st.enter_context(nc.sbuf_tensor("s_" + name, list(shape), dt))

            A1 = sb("A1", [128, 52224], BF16)
            A2 = sb("A2", [128, 20480], BF16)
            idb = sb("idb", [128, 128], BF16)
            idf = sb("idf", [128, 128], F32)
            onesb = sb("onesb", [128, 128], BF16)
            modT = sb("modT", [128, 48, 5], F32)
            G1p = sb("G1p", [128, D], BF16)
            G2p = sb("G2p", [128, D], BF16)
            G1s = sb("G1s", [32, D], BF16)
            G2s = sb("G2s", [32, D], BF16)
            EB = sb("EB", [128, 5, 4, 128], BF16)
            bsSB = sb("bsSB", [128, 128], F32)
            wts = sb("wts", [128, 4, 128], BF16)
            wtsS = sb("wtsS", [32, 4, 32], BF16)
            wrt = sb("wrt", [128, 8, 20], F32)
            brB = sb("brB", [128, 20], F32)
            sml = sb("sml", [128, 64], F32)
            ssb = sb("ssb", [128, 64], F32)
            xt = [sb("xt%d" % i, [128, D], F32) for i in range(2)]
            sq = sb("sq", [128, D], BF16)
            xn = [sb("xn%d" % i, [128, D], BF16) for i in range(4)]
            W3 = sb("W3", [128, 13312], BF16)

            QT = A1[:, 0:8192].rearrange("p (h t) -> p h t", h=4)
            SGT = A1[:, 8192:16384].rearrange("p (g t) -> p g t", g=4)
            KT = A1[:, 16384:32768].rearrange("p (h t) -> p h t", h=4)
            VS = A1[:, 32768:49152].rearrange("p (b c) -> p b c", b=32)
            TAIL = A1[:, 49152:52224].bitcast(F32)
            gsgB = TAIL[:, 0:512]
            bsgB = TAIL[:, 512:1024]
            bsB = TAIL[:, 1024:1536]
            H2T = A1[:, 16384:16384 + 8 * 2176].rearrange("p (k t) -> p k t", k=8)
            yaccA = A1[:, 0:16384].bitcast(F32).rearrange("p (b d) -> p b d", d=D)
            yaccB = A1[:, 33792:33792 + 18432].bitcast(F32).rearrange("p (b d) -> p b d", d=D)

            def yacc(blk):
                return yaccA[:, blk, :] if blk < 8 else yaccB[:, blk - 8, :]

            WIN = A2[:, 0:20480].rearrange("p (k n) -> p k n", k=8)
            WO = A2[:, 0:8192].rearrange("p (k n) -> p k n", k=8)
            ESLOT = [A2[:, i * 4096:(i + 1) * 4096] for i in range(4)]
            RTALL = A2[:, 16384:16384 + 2048].bitcast(F32)
            LG = RTALL[:, 0:340].rearrange("p (b n) -> p b n", b=17)
            COMB = RTALL[:, 340:612].rearrange("p (b n) -> p b n", b=17)
            RT = RTALL[:, 612:1020].rearrange("p (b n) -> p b n", b=17)
            hT = W3[:, 0:4096].rearrange("p (k t) -> p k t", k=8)
            kst = W3[:, 4096:5120].bitcast(F32)
            vst = W3[:, 5120:6144].bitcast(F32)
            k16 = W3[:, 6144:6656]
            uT = W3[:, 6656:7680].rearrange("p (g t) -> p g t", g=4)
            vln = W3[:, 7680:8704].bitcast(F32)
            vlb = W3[:, 8704:9216]
            sqv = W3[:, 9216:10240].bitcast(F32)
            gtmp = W3[:, 10240:11264].bitcast(F32)
            wach = [W3[:, 4096 + i * 2048: 4096 + (i + 1) * 2048].rearrange("p (k n) -> p k n", k=8) for i in range(2)]
            mrow = W3[:, 0:512].bitcast(F32)
            bac = W3[:, 512:1024].bitcast(F32)
            PT = [W3[:, 11264 + i * 512: 11264 + (i + 1) * 512] for i in range(4)]
            ep_rl = W3[:, 0:1024].bitcast(F32)
            ep_on0 = W3[:, 1024:2048].bitcast(F32)
            ep_on1 = W3[:, 2048:3072].bitcast(F32)
            ep_o = W3[:, 3072:4096].bitcast(F32)
            ep_sq = W3[:, 4096:4608]
            ep_r = W3[:, 4608:5632].bitcast(F32)
            h2Tf = W3[:, 0:2048].bitcast(F32).rearrange("p (k t) -> p k t", k=8)
            x1b = W3[:, 2048:4096].bitcast(F32)
            xnf = W3[:, 4096:6144].bitcast(F32)
            tmpf = W3[:, 6144:8192].bitcast(F32)
            he = [W3[:, 8192 + i * 2048: 8192 + (i + 1) * 2048].rearrange("p (f t) -> p f t", f=4) for i in range(2)]
            sil = W3[:, 12288:13312].bitcast(F32)

            pb = [st.enter_context(nc.psum_tensor("pb%d" % i, [128, 512], F32)) for i in range(8)]
            pbi = [0]

            nbanks = [8]

            def nextbank():
                b = pb[pbi[0] % nbanks[0]]
                pbi[0] += 1
                return b

            ssi = [0]

            def sscols(n=4):
                i = ssi[0] % 16
                ssi[0] += 1
                return ssb[:, i * 4:(i * 4 + n)]

            evi = [0]

            def evac_eng():
                evi[0] += 1
                return 'dve' if evi[0] % 2 else 'act'

            def affine(eng, out, in_, s_ap, b_ap):
                if eng == 'act':
                    P.act(out, in_, AF.Identity, bias=b_ap, scale=s_ap)
                else:
                    P.ts(eng, out, in_, s_ap, b_ap, op0=ALU.mult, op1=ALU.add)

            def pcopy(eng, out, in_):
                if eng == 'act':
                    P.act(out, in_, AF.Copy)
                else:
                    P.copy(eng, out, in_)

            P.dma('sp', idb[:], ident_in)
            P.dma('sp', idf[:], identf_in)
            P.memset('dve', onesb[:], 1.0)
            P.dma('sp', gsgB, gsg_in.to_broadcast([128, 512]))
            P.dma('sp', bsgB, bsg_in.to_broadcast([128, 512]))
            P.dma('sp', bsB, bs_in.to_broadcast([128, 512]))
            P.dma('sp', bsSB[:], bsS_in.to_broadcast([128, 128]))
            P.dma('sp', brB[:], br_in.to_broadcast([128, 20]))
            P.dma('sp', wrt[:], wr_in.rearrange("(k p) n -> p k n", p=128))
            P.dma('sp', sml[:, 8:9], gsub_in)
            P.dma('sp', sml[:, 12:16], b31_in.to_broadcast([128, 4]))
            lamt = gtmp[:, 0:256]
            P.dma('sp', lamt, lam4.to_broadcast([128, 256]))
            P.tt('dve', sqv[:, 0:64], lamt[:, 0:64], lamt[:, 64:128], ALU.mult)
            P.tt('dve', sqv[:, 64:128], lamt[:, 128:192], lamt[:, 192:256], ALU.mult)
            P.red('dve', sml[:, 0:2], sqv[:, 0:128].rearrange("p (a b) -> p a b", a=2), ALU.add)
            P.act(sml[:, 2:4], sml[:, 0:2], AF.Exp)
            P.tt('dve', sml[:, 4:5], sml[:, 3:4], sml[:, 2:3], ALU.subtract)
            P.ts('dve', sml[:, 5:6], sml[:, 4:5], -LAM_INIT, None, op0=ALU.add)
            neglam = sml[:, 5:6]
            P.ts('dve', sml[:, 9:10], sml[:, 8:9], 1.0 - LAM_INIT, None, op0=ALU.mult)
            gsubc = sml[:, 9:10]
            P.dma('sp', sqv[:, 0:512], wsT_in)
            P.dma('sp', gtmp[:, 0:128], tril_in)
            for g in range(4):
                P.tt('dve', wts[:, g, :], sqv[:, g * 128:(g + 1) * 128], gtmp[:, 0:128], ALU.mult)
            P.dma('sp', vln[0:32, 0:128], wsS_in)
            P.dma('sp', vln[0:32, 128:160], trilS_in)
            for g in range(4):
                P.tt('dve', wtsS[:, g, :], vln[0:32, g * 32:(g + 1) * 32], vln[0:32, 128:160], ALU.mult)

            cint = xt[0][0:5, :]
            P.dma('sp', cint, cin)
            scs = xt[1][0:5, :]
            P.act(scs, cint, AF.Silu)
            b0 = nextbank()
            for kc in range(8):
                P.tr(b0[:, kc * 5:(kc + 1) * 5], scs[:, kc * 128:(kc + 1) * 128], idf[0:5, 0:5])
            scT = sq[:, 0:40].rearrange("p (k r) -> p k r", k=8)
            P.copy('dve', scT, b0[:, 0:40].rearrange("p (k r) -> p k r", k=8))
            selt = sml[0:5, 16:16 + 0]
            selsb = sb("selsb", [5, 160], F32)
            P.dma('sp', selsb[:], sel_in)
            for n in range(24):
                wa = wach[n % 2]
                P.dma('pool', wa, w_ada[:, n * 256:(n + 1) * 256].rearrange("(k p) n -> p k n", p=128))
                P.dma('sp', bac[0:5, 0:256], b_ada[:, n * 256:(n + 1) * 256].to_broadcast([5, 256]))
                bk = nextbank()
                for kc in range(8):
                    P.mm(bk[0:5, 0:256], scT[:, kc, :], wa[:, kc, :], start=(kc == 0), stop=(kc == 7))
                P.tt('dve', mrow[0:5, 0:256], bk[0:5, 0:256], bac[0:5, 0:256], ALU.add)
                bk2 = nextbank()
                for j in range(2):
                    P.tr(bk2[:, j * 5:(j + 1) * 5], mrow[0:5, j * 128:(j + 1) * 128], idf[0:5, 0:5])
                P.copy('dve', modT[:, n * 2:(n + 1) * 2, :], bk2[:, 0:10].rearrange("p (k r) -> p k r", k=2))
                part = n // 4
                if part in (2, 5):
                    Gp, Gs = (G1p, G1s) if part == 2 else (G2p, G2s)
                    c0 = (n % 4) * 256
                    bk3 = nextbank()
                    P.mm(bk3[:, 0:256], selsb[0:5, 0:128], mrow[0:5, 0:256])
                    P.copy('dve', Gp[:, c0:c0 + 256], bk3[:, 0:256])
                    bk4 = nextbank()
                    P.mm(bk4[0:32, 0:256], selsb[0:5, 128:160], mrow[0:5, 0:256])
                    P.copy('dve', Gs[:, c0:c0 + 256], bk4[0:32, 0:256])
            for ch0 in (8, 32):
                P.ts('dve', modT[:, ch0:ch0 + 8, :], modT[:, ch0:ch0 + 8, :], 1.0, None, op0=ALU.add)

            ebsrc = [xt[0][:, :], xt[1][:, :]]
            P.dma('sp', xt[0][:, 0:1024], biasg[:, 0:1024])
            P.dma('sp', xt[1][:, 0:1024], biasg[:, 1024:2048])
            P.dma('sp', xnf[:, 0:512], biasg[:, 2048:2560])
            P.dma('sp', tmpf[:, 0:640], maskg)
            P.ts('dve', sml[:, 16:20], sml[:, 12:16], -1.0, None, op0=ALU.mult)
            for i in range(5):
                for h in range(4):
                    col = (i * 4 + h) * 128
                    src = (xt[0][:, col:col + 128] if col < 1024 else
                           xt[1][:, col - 1024:col - 1024 + 128] if col < 2048 else xnf[:, col - 2048:col - 2048 + 128])
                    P.act(x1b[:, 0:128], src, AF.Exp, bias=sml[:, 16 + h:17 + h], scale=1.0)
                    P.tt('dve', EB[:, i, h, :], x1b[:, 0:128], tmpf[:, i * 128:(i + 1) * 128], ALU.mult)

            if STAGE <= 1:
                raise _Stop()
            for k in range(8):
                P.dma('pool', WIN[:, k, :], w_in[k * 128:(k + 1) * 128, :])

            def norm_rows(x_ap, rows, out_ap):
                s = sscols(4)
                P.act(sq[0:rows, :], x_ap, AF.Square, accum_out=s[0:rows, 0:1])
                P.act(s[0:rows, 1:2], s[0:rows, 0:1], AF.Sqrt, bias=EPS, scale=1.0 / D)
                P.recip(s[0:rows, 2:3], s[0:rows, 1:2])
                P.ts('dve', out_ap, x_ap, s[0:rows, 2:3], None, op0=ALU.mult)

            def ln_groups(ps, rows, out_f32, out_bf):
                s = sscols(4)
                s2 = sscols(4)
                s3 = sscols(4)
                s4 = sscols(4)
                pv = ps.rearrange("p (g c) -> p g c", g=4)
                P.red('dve', s[0:rows, :], pv, ALU.add)
                P.act(sqv[0:rows, :], ps, AF.Square)
                P.red('dve', s2[0:rows, :], sqv[0:rows, :].rearrange("p (g c) -> p g c", g=4), ALU.add)
                P.ts('dve', s[0:rows, :], s[0:rows, :], 1.0 / 128, None, op0=ALU.mult)
                P.tt('dve', s3[0:rows, :], s[0:rows, :], s[0:rows, :], ALU.mult)
                P.stt('dve', s2[0:rows, :], s2[0:rows, :], 1.0 / 128, s3[0:rows, :], ALU.mult, ALU.subtract)
                P.act(s3[0:rows, :], s2[0:rows, :], AF.Sqrt, bias=EPS, scale=1.0)
                P.recip(s4[0:rows, :], s3[0:rows, :])
                P.stt('dve', s3[0:rows, :], s[0:rows, :], -1.0, s4[0:rows, :], ALU.mult, ALU.mult)
                for g in range(4):
                    P.ts('dve', gtmp[0:rows, g * 128:(g + 1) * 128], ps[:, g * 128:(g + 1) * 128],
                         s4[0:rows, g:g + 1], s3[0:rows, g:g + 1], op0=ALU.mult, op1=ALU.add)
                P.tt('dve', gtmp[0:rows, :], gtmp[0:rows, :], gsgB[0:rows, :], ALU.mult)
                P.tt('dve', out_f32, gtmp[0:rows, :], bsgB[0:rows, :], ALU.add)
                P.copy('dve', out_bf, out_f32)

            for g in range(int(os.environ.get('MK_NG', '8'))):
                for j in range(4):
                    blk = g * 4 + j
                    xb = xt[blk % 2]
                    P.dma('sp', xb[:], xp[blk * 128:(blk + 1) * 128, :])
                    norm_rows(xb[:], 128, xn[j][:])
                _chk(1)
                for dkp in range(4):
                    bk = nextbank()
                    pT = bk[:].bitcast(BF16)
                    for d2 in range(2):
                        dk = dkp * 2 + d2
                        for j in range(4):
                            P.tr(pT[:, d2 * 512 + j * 128: d2 * 512 + (j + 1) * 128], xn[j][:, dk * 128:(dk + 1) * 128], idb[:])
                    for d2 in range(2):
                        dk = dkp * 2 + d2
                        affine(evac_eng(), hT[:, dk, :], pT[:, d2 * 512:(d2 + 1) * 512], modT[:, 8 + dk, 0:1], modT[:, dk, 0:1])
                _chk(2)
                for j in range(4):
                    pblk = g * 4 + j
                    own = j < 2
                    orow = (2 * g + j) * 128
                    bk = nextbank()
                    for dk in range(8):
                        P.mm(bk[:], hT[:, dk, j * 128:(j + 1) * 128], WIN[:, dk, 512:1024], start=(dk == 0), stop=(dk == 7))
                    if own:
                        P.act(kst, bk[:], AF.Copy)
                        P.dma('sp', kp[orow:orow + 128, :], kst)
                    P.copy('dve', k16, bk[:])
                    bk2 = nextbank()
                    pKT = bk2[:].bitcast(BF16)
                    for h in range(4):
                        P.tr(pKT[:, h * 128:(h + 1) * 128], k16[:, h * 128:(h + 1) * 128], idb[:])
                    pcopy(evac_eng(), KT[:, :, pblk * 128:(pblk + 1) * 128], pKT[:, 0:512].rearrange("p (h t) -> p h t", h=4))
                    bk = nextbank()
                    for dk in range(8):
                        P.mm(bk[:], hT[:, dk, j * 128:(j + 1) * 128], WIN[:, dk, 1024:1536], start=(dk == 0), stop=(dk == 7))
                    if own:
                        P.act(vst, bk[:], AF.Copy)
                        P.dma('sp', vp[orow:orow + 128, :], vst)
                    P.copy('dve', VS[:, pblk, :], bk[:])
                _chk(3)
                t0 = 2 * g * 128
                for hh in range(2):
                    bk = nextbank()
                    for h2 in range(2):
                        h = hh * 2 + h2
                        for dk in range(8):
                            P.mm(bk[:, h2 * 256:(h2 + 1) * 256], WIN[:, dk, h * 128:(h + 1) * 128], hT[:, dk, 0:256],
                                 start=(dk == 0), stop=(dk == 7))
                    pcopy(evac_eng(), QT[:, hh * 2:hh * 2 + 2, t0:t0 + 256], bk[:].rearrange("p (h t) -> p h t", h=2))
                for hh in range(2):
                    bk = nextbank()
                    for h2 in range(2):
                        gi = hh * 2 + h2
                        for dk in range(8):
                            P.mm(bk[:, h2 * 256:(h2 + 1) * 256], WIN[:, dk, 1536 + gi * 128:1536 + (gi + 1) * 128], hT[:, dk, 0:256],
                                 start=(dk == 0), stop=(dk == 7))
                    pcopy(evac_eng(), uT[:, hh * 2:hh * 2 + 2, :], bk[:].rearrange("p (h t) -> p h t", h=2))
                for j in range(2):
                    bk = nextbank()
                    for dk in range(8):
                        P.mm(bk[:], hT[:, dk, j * 128:(j + 1) * 128], WIN[:, dk, 2048:2560], start=(dk == 0), stop=(dk == 7))
                    ln_groups(bk[:], 128, vln, vlb)
                    bk2 = nextbank()
                    for gi in range(4):
                        P.mm(bk2[:, gi * 128:(gi + 1) * 128], vlb[:, gi * 128:(gi + 1) * 128], wts[:, gi, :])
                    P.tt('dve', gtmp, bk2[:], bsB, ALU.add)
                    tt0 = t0 + j * 128
                    P.tt('dve', SGT[:, :, tt0:tt0 + 128], gtmp.rearrange("p (g t) -> p g t", g=4),
                         uT[:, :, j * 128:(j + 1) * 128], ALU.mult)

            if os.environ.get('MK_NOS'):
                raise _Stop()
            hTs = hT[:, :, 0:32]
            xsb = xt[0][0:32, :]
            P.dma('sp', xsb, xs)
            norm_rows(xsb, 32, xn[0][0:32, :])
            bk = nextbank()
            pT = bk[:].bitcast(BF16)
            for dk in range(8):
                P.tr(pT[:, dk * 32:(dk + 1) * 32], xn[0][0:32, dk * 128:(dk + 1) * 128], idb[0:32, 0:32])
            for dk in range(8):
                for s in range(4):
                    affine('dve', hT[:, dk, s * 8:(s + 1) * 8], pT[:, dk * 32 + s * 8: dk * 32 + (s + 1) * 8],
                           modT[:, 8 + dk, 1 + s:2 + s], modT[:, dk, 1 + s:2 + s])
            SK = sb("SK", [128, 4, 32], BF16)
            SVs = [xn[1 + s_ // 2][0:8, (s_ % 2) * 512:(s_ % 2 + 1) * 512] for s_ in range(4)]
            SQ = sb("SQ", [128, 4, 32], BF16)
            SGS = sb("SGS", [128, 4, 32], BF16)
            bk = nextbank()
            for dk in range(8):
                P.mm(bk[0:32, :], hT[:, dk, 0:32], WIN[:, dk, 512:1024], start=(dk == 0), stop=(dk == 7))
            P.act(kst[0:32, :], bk[0:32, :], AF.Copy)
            P.dma('sp', ks, kst[0:32, :])
            P.copy('dve', k16[0:32, :], bk[0:32, :])
            bk2 = nextbank()
            pKT = bk2[:].bitcast(BF16)
            for h in range(4):
                P.tr(pKT[:, h * 32:(h + 1) * 32], k16[0:32, h * 128:(h + 1) * 128], idb[0:32, 0:32])
            P.copy('dve', SK[:], pKT[:, 0:128].rearrange("p (h t) -> p h t", h=4))
            bk = nextbank()
            for dk in range(8):
                P.mm(bk[0:32, :], hT[:, dk, 0:32], WIN[:, dk, 1024:1536], start=(dk == 0), stop=(dk == 7))
            P.act(vst[0:32, :], bk[0:32, :], AF.Copy)
            P.dma('sp', vso, vst[0:32, :])
            for s_ in range(4):
                bk = nextbank()
                for dk in range(8):
                    P.mm(bk[0:8, :], hT[:, dk, s_ * 8:(s_ + 1) * 8], WIN[:, dk, 1024:1536], start=(dk == 0), stop=(dk == 7))
                P.copy('dve', SVs[s_], bk[0:8, :])
            bk = nextbank()
            for h in range(4):
                for dk in range(8):
                    P.mm(bk[:, h * 32:(h + 1) * 32], WIN[:, dk, h * 128:(h + 1) * 128], hT[:, dk, 0:32], start=(dk == 0), stop=(dk == 7))
            P.copy('dve', SQ[:], bk[:, 0:128].rearrange("p (h t) -> p h t", h=4))
            bk = nextbank()
            for gi in range(4):
                for dk in range(8):
                    P.mm(bk[:, gi * 32:(gi + 1) * 32], WIN[:, dk, 1536 + gi * 128:1536 + (gi + 1) * 128], hT[:, dk, 0:32],
                         start=(dk == 0), stop=(dk == 7))
            P.copy('dve', uT[:, :, 0:32], bk[:, 0:128].rearrange("p (h t) -> p h t", h=4))
            bk = nextbank()
            for dk in range(8):
                P.mm(bk[0:32, :], hT[:, dk, 0:32], WIN[:, dk, 2048:2560], start=(dk == 0), stop=(dk == 7))
            ln_groups(bk[0:32, :], 32, vln[0:32, :], vlb[0:32, :])
            P.dma('sp', sgv, vln[0:32, :])
            bk2 = nextbank()
            for gi in range(4):
                P.mm(bk2[:, gi * 32:(gi + 1) * 32], vlb[0:32, gi * 128:(gi + 1) * 128], wtsS[0:32, gi, :])
            P.tt('dve', gtmp[:, 0:128], bk2[:, 0:128], bsSB[:], ALU.add)
            P.tt('dve', SGS[:], gtmp[:, 0:128].rearrange("p (g t) -> p g t", g=4), uT[:, :, 0:32], ALU.mult)

            if STAGE <= 2:
                raise _Stop()
            P.memset('dve', sml[:, 20:22], 0.0)
            for (src, nch, col) in ((QT, 4, 20), (KT, 8, 21)):
                for h in range(4):
                    for c in range(nch):
                        P.act(PT[0], src[:, h, c * 512:(c + 1) * 512], AF.Square)
                        bk = nextbank()
                        P.mm(bk[:], onesb[:], PT[0])
                        s = sscols(4)
                        P.red('dve', s[:, 0:1], bk[:], ALU.max)
                        P.tt('dve', sml[:, col:col + 1], sml[:, col:col + 1], s[:, 0:1], ALU.max)
            P.tt('dve', sml[:, 22:23], sml[:, 20:21], sml[:, 21:22], ALU.add)
            P.ts('dve', sml[:, 23:24], sml[:, 22:23], -1.02 / 16.0, None, op0=ALU.mult)
            negC = sml[:, 23:24]

            PS = [pb[0], pb[1], pb[2], pb[3]]
            PO = [pb[4], pb[5]]
            PL = [pb[6], pb[7]]
            it = [0]
            for c in range(4):
                for h in range(4):
                    kbs = []
                    for u in range(4 * c + 4):
                        for pos in range(2):
                            r = max(u - 4 * c, 0)
                            sp_list = []
                            if pos == 0:
                                if u >= 4 * c:
                                    sp_list.append(((u - 4 * c) * 128, 0))
                                if u + 1 >= 4 * c and u + 1 < 4 * c + 4:
                                    sp_list.append(((u + 1 - 4 * c) * 128, 3 + (u + 1) % 2))
                            else:
                                if u >= 4 * c:
                                    sp_list.append(((u - 4 * c) * 128, 1 + u % 2))
                            kbs.append((_pblk(u, pos), r * 128, sp_list))
                    nk = len(kbs)
                    base_it = it[0]

                    def s_stage(ki):
                        pblk, col0, sp_list = kbs[ki]
                        par = (base_it + ki) % 2
                        for m in range(2):
                            ps_ = PS[par * 2 + m]
                            P.mm(ps_[:, col0:512], KT[m * 64:(m + 1) * 64, h, pblk * 128:(pblk + 1) * 128],
                                 QT[m * 64:(m + 1) * 64, h, c * 512 + col0:(c + 1) * 512])
                        for m in range(2):
                            ps_ = PS[par * 2 + m]
                            pt_ = PT[par * 2 + m]
                            P.act(pt_[:, col0:512], ps_[:, col0:512], AF.Exp, bias=negC, scale=0.125)
                            for (a, idx) in sp_list:
                                P.tt('dve', pt_[:, a:a + 128], pt_[:, a:a + 128], EB[:, idx, h, :], ALU.mult)

                    def av_stage(ki):
                        pblk, col0, sp_list = kbs[ki]
                        par = (base_it + ki) % 2
                        for m in range(2):
                            pt_ = PT[par * 2 + m]
                            P.mm(PO[m][:, col0:512], VS[:, pblk, h * 128:(h + 1) * 128], pt_[:, col0:512],
                                 start=(ki == 0), stop=(ki == nk - 1))
                            P.mm(PL[m][:, col0:512], onesb[:], pt_[:, col0:512], start=(ki == 0), stop=(ki == nk - 1))

                    s_stage(0)
                    for ki in range(1, nk):
                        s_stage(ki)
                        av_stage(ki - 1)
                    av_stage(nk - 1)
                    it[0] += nk
                    for m, on in ((0, ep_on0), (1, ep_on1)):
                        P.recip(ep_rl, PL[m][:])
                        P.tt('dve', on, PO[m][:], ep_rl, ALU.mult)
                    P.stt('dve', ep_o, ep_on1, neglam, ep_on0, ALU.mult, ALU.add)
                    P.act(ep_sq, ep_o, AF.Square)
                    bx = PS[0]
                    P.mm(bx[:], onesb[:], ep_sq)
                    P.act(ep_r, bx[:], AF.Sqrt, bias=EPS, scale=1.0 / 128)
                    P.recip(ep_rl, ep_r)
                    P.stt('dve', QT[:, h, c * 512:(c + 1) * 512], ep_o, gsubc, ep_rl, ALU.mult, ALU.mult)
            AOT = QT

            if STAGE <= 3:
                raise _Stop()
            for k in range(8):
                P.dma('pool', WO[:, k, :], w_o[k * 128:(k + 1) * 128, :])

            def b2_block(blk, rows, mT_chunks, x_src_ap, Gt, G2unused=None):
                xb = xt[blk % 2]
                P.dma('sp', xb[0:rows, :], x_src_ap)
                for half in range(2):
                    bk = nextbank()
                    for fc in range(8):
                        P.mm(bk[0:rows, :], mT_chunks[fc], WO[:, fc, half * 512:(half + 1) * 512], start=(fc == 0), stop=(fc == 7))
                    P.tt('dve', tmpf[0:rows, half * 512:(half + 1) * 512], bk[0:rows, :], Gt[0:rows, half * 512:(half + 1) * 512], ALU.mult)
                P.tt('dve', x1b[0:rows, :], tmpf[0:rows, :], xb[0:rows, :], ALU.add)
                P.dma('sp', x1scr[blk * 128:blk * 128 + rows, :], x1b[0:rows, :])
                norm_rows(x1b[0:rows, :], rows, xnf[0:rows, :])
                for hf in range(2):
                    bk = nextbank()
                    for d4 in range(4):
                        dk = hf * 4 + d4
                        P.tr(bk[:, d4 * 128:d4 * 128 + rows], xnf[0:rows, dk * 128:(dk + 1) * 128], idf[0:rows, 0:rows])
                    for d4 in range(4):
                        dk = hf * 4 + d4
                        if rows == 128:
                            affine(evac_eng(), h2Tf[:, dk, :], bk[:, d4 * 128:(d4 + 1) * 128], modT[:, 32 + dk, 0:1], modT[:, 24 + dk, 0:1])
                        else:
                            for s in range(4):
                                affine('dve', h2Tf[:, dk, s * 8:(s + 1) * 8], bk[:, d4 * 128 + s * 8:d4 * 128 + (s + 1) * 8],
                                       modT[:, 32 + dk, 1 + s:2 + s], modT[:, 24 + dk, 1 + s:2 + s])
                P.copy('dve', H2T[:, :, blk * 128:blk * 128 + rows], h2Tf[:, :, 0:rows])
                bk = nextbank()
                for dk in range(8):
                    P.mm(bk[0:rows, 0:20], h2Tf[:, dk, 0:rows], wrt[:, dk, :], start=(dk == 0), stop=(dk == 7))
                P.tt('dve', LG[0:rows, blk, :], bk[0:rows, 0:20], brB[0:rows, :], ALU.add)

            for t in range(16):
                chunks = [AOT[:, h, t * 128:(t + 1) * 128] for h in range(4)] + [SGT[:, g, t * 128:(t + 1) * 128] for g in range(4)]
                prow = _pblk(t, 0) * 128
                b2_block(t, 128, chunks, xp[prow:prow + 128, :], G1p)

            if STAGE <= 4:
                raise _Stop()
            SAO = sb("SAO", [128, 4, 32], BF16)
            if do_sample:

                S_all = A1[:, 33792:33792 + 16400].bitcast(F32)
                Pm = A1[:, 0:8200]
                gt_f = [W3[:, i * 4096:(i + 1) * 4096].bitcast(F32).rearrange("p (r d) -> p r d", r=4) for i in range(2)]
                gt_b = [W3[:, 8192 + i * 2048: 8192 + (i + 1) * 2048].rearrange("p (r d) -> p r d", r=4) for i in range(2)]
                KTp = [A2[:, 8192 + i * 512: 8192 + (i + 1) * 512].rearrange("p (h t) -> p h t", h=4) for i in range(2)]
                PTp = [A2[:, 9216 + i * 128: 9216 + (i + 1) * 128] for i in range(2)]
                idx = A2[:, 9472:9600].bitcast(I32)
                ptb = A2[:, 9984:10112].bitcast(I32)
                SBT = A2[:, 10496:10768].bitcast(F32)
                smk = A2[:, 10768:11040].bitcast(F32)
                dsl = A2[:, 11040:11168].bitcast(F32)
                Dm = A2[:, 11168:11232].bitcast(F32)
                On = A2[:, 11232:11488].bitcast(F32)
                osb = A2[:, 11488:11744].bitcast(F32)
                PTn = A2[:, 11744:11872]
                sm2 = A2[:, 11872:11936].bitcast(F32)
                QB = xn[3][:, 0:512].rearrange("p (s h c) -> p s h c", s=4, h=4)
                ck4 = cache_k.rearrange("(a r) d -> a (r d)", r=4)
                cv4 = cache_v.rearrange("(a r) d -> a (r d)", r=4)
                P.dma('sp', ptb, pt_in)
                P.dma('sp', sm2[:, 0:1], pidx_in)
                P.dma('sp', sm2[:, 1:2], sb31_in)
                P.dma('sp', SBT, sbias_in)
                P.dma('sp', smk, smask_in)
                P.dma('sp', dsl, dsel_in)
                P.ts('dve', idx, ptb, 32.0, sm2[:, 0:1], op0=ALU.mult, op1=ALU.add)
                P.ts('dve', SBT, SBT, sm2[:, 1:2], None, op0=ALU.subtract)
                P.tt('dve', SBT, SBT, smk, ALU.add)
                P.stt('dve', Dm, dsl[:, 32:64], neglam, dsl[:, 0:32], ALU.mult, ALU.add)
                P.memset('dve', QB, 0.0)
                for h in range(4):
                    P.copy('dve', QB[0:64, :, h, 0:8], SQ[0:64, h, :].rearrange("p (s q) -> p s q", s=4))
                    P.copy('dve', QB[64:128, :, h, 8:16], SQ[64:128, h, :].rearrange("p (s q) -> p s q", s=4))
                nbanks[0] = 7
                bO = pb[7]

                def gather(dst, table, col):
                    off = bass.IndirectOffsetOnAxis(ap=idx[:, col:col + 1], axis=0)
                    P.custom('pool', 'sw',
                             lambda e: e.indirect_dma_start(out=dst, out_offset=None, in_=table, in_offset=off),
                             [idx[:, col:col + 1]], [dst])

                def tp(h):
                    return dict(tile_position=(0, 96)) if h == 3 else {}

                gi = [0]
                ki2 = [0]
                for s_ in range(4):
                    ktps = {}
                    bSs = {}
                    cur = {}

                    def k_stage(t):
                        G, r = t // 4, t % 4
                        if r == 0:
                            gf = gt_f[gi[0] % 2]
                            gb = gt_b[gi[0] % 2]
                            gi[0] += 1
                            gather(gf.rearrange("p r d -> p (r d)"), ck4, s_ * 16 + G)
                            P.copy('dve', gb, gf)
                            cur['b'] = gb
                        gb = cur['b']
                        ktp = KTp[ki2[0] % 2]
                        ki2[0] += 1
                        bkT = nextbank()
                        bkv = bkT[:].bitcast(BF16)
                        for h in range(4):
                            P.tr(bkv[:, h * 128:(h + 1) * 128], gb[:, r, h * 128:(h + 1) * 128], idb[:])
                        P.act(ktp, bkv[:, 0:512].rearrange("p (h t) -> p h t", h=4), AF.Copy)
                        ktps[t] = ktp

                    def sc_stage(t):
                        G, r = t // 4, t % 4
                        ktp = ktps.pop(t)
                        if r == 0:
                            bSs[G] = nextbank()
                        bS_ = bSs[G]
                        for h in range(4):
                            P.mm(bS_[32 * h:32 * h + 32, r * 128:(r + 1) * 128], QB[:, s_, h, :], ktp[:, h, :], **tp(h))
                        if r == 3:
                            P.act(S_all[:, G * 512:(G + 1) * 512], bS_[:], AF.Copy, scale=0.125)
                            del bSs[G]

                    k_stage(0)
                    for t in range(1, 64):
                        k_stage(t)
                        sc_stage(t - 1)
                    sc_stage(63)
                    bS = nextbank()
                    for h in range(4):
                        P.mm(bS[32 * h:32 * h + 32, 0:8], QB[:, s_, h, :], SK[:, h, s_ * 8:(s_ + 1) * 8], **tp(h))
                    P.act(S_all[:, 8192:8200], bS[:, 0:8], AF.Copy, scale=0.125)
                    lastv = S_all[:, 7680:8192].rearrange("p (r c) -> p r c", r=4)[:, :, 96:128]
                    P.tt('dve', lastv, lastv, SBT[:, 0:128].rearrange("p (r c) -> p r c", r=4), ALU.add)
                    P.tt('dve', S_all[:, 8192:8200], S_all[:, 8192:8200], SBT[:, 128:136], ALU.add)
                    P.red('dve', sm2[:, 4:5], S_all, ALU.max)
                    P.ts('dve', sm2[:, 5:6], sm2[:, 4:5], -1.0, None, op0=ALU.mult)
                    P.act(Pm, S_all, AF.Exp, bias=sm2[:, 5:6], scale=1.0, accum_out=sm2[:, 6:7])
                    P.recip(sm2[:, 7:8], sm2[:, 6:7])
                    pv = {}

                    def p_stage(t):
                        G, r = t // 4, t % 4
                        if r == 0:
                            gf = gt_f[gi[0] % 2]
                            gb = gt_b[gi[0] % 2]
                            gi[0] += 1
                            gather(gf.rearrange("p r d -> p (r d)"), cv4, s_ * 16 + G)
                            P.copy('dve', gb, gf)
                            cur['b'] = gb
                        gb = cur['b']
                        ptp = PTp[ki2[0] % 2]
                        ki2[0] += 1
                        bP = nextbank()
                        bPv = bP[:].bitcast(BF16)
                        P.tr(bPv[:, 0:128], Pm[:, t * 128:(t + 1) * 128], idb[:])
                        P.act(ptp, bPv[:, 0:128], AF.Copy)
                        pv[t] = (ptp, gb, r)

                    def av2_stage(t):
                        ptp, gb, r = pv.pop(t)
                        for h in range(4):
                            P.mm(bO[32 * h:32 * h + 32, 0:128], ptp[:, 32 * h:32 * h + 32], gb[:, r, h * 128:(h + 1) * 128],
                                 start=(t == 0), stop=False, **tp(h))

                    p_stage(0)
                    for t in range(1, 64):
                        p_stage(t)
                        av2_stage(t - 1)
                    av2_stage(63)
                    bP = nextbank()
                    bPv = bP[:].bitcast(BF16)
                    P.tr(bPv[0:8, 0:128], Pm[:, 8192:8200], idb[:])
                    P.act(PTn[0:8, :], bPv[0:8, 0:128], AF.Copy)
                    for h in range(4):
                        P.mm(bO[32 * h:32 * h + 32, 0:128], PTn[0:8, 32 * h:32 * h + 32], SVs[s_][:, h * 128:(h + 1) * 128],
                             start=False, stop=True, **tp(h))
                    P.ts('dve', On, bO[:, 0:128], sm2[:, 7:8], None, op0=ALU.mult)
                    bC = nextbank()
                    P.mm(bC[0:32, 0:128], Dm, On)
                    P.act(sq[0:32, 0:128], bC[0:32, 0:128], AF.Square, accum_out=sm2[0:32, 8:9])
                    P.act(sm2[0:32, 9:10], sm2[0:32, 8:9], AF.Sqrt, bias=EPS, scale=1.0 / 128)
                    P.recip(sm2[0:32, 10:11], sm2[0:32, 9:10])
                    P.ts('dve', osb[0:32, :], bC[0:32, 0:128], sm2[0:32, 10:11], None, op0=ALU.mult)
                    bT = nextbank()
                    P.tr(bT[:, 0:32], osb[0:32, :], idf[0:32, 0:32])
                    P.ts('dve', SAO[:, :, s_ * 8:(s_ + 1) * 8], bT[:, 0:32].rearrange("p (h q) -> p h q", h=4), gsubc, None, op0=ALU.mult)
                nbanks[0] = 8
            else:
                P.memset('dve', SAO[:], 0.0)
            chunks = [SAO[:, h, :] for h in range(4)] + [SGS[:, g, :] for g in range(4)]
            P.memset('dve', LG[:, 16, :], 0.0)
            b2_block(16, 32, chunks, xs, G1s)

            if STAGE <= 5:
                raise _Stop()
            router(P, LG, COMB, RT)
            gfinA = xn[0][:].bitcast(F32)
            gfinB = xn[1][:].bitcast(F32)
            P.dma('sp', gfinA, gfin_in[:, 0:512].to_broadcast([128, 512]))
            P.dma('sp', gfinB, gfin_in[:, 512:1024].to_broadcast([128, 512]))

            def finalize(blk):
                rows = 128 if blk < 16 else 32
                Gt = G2p if blk < 16 else G2s
                xb = xt[blk % 2]
                P.dma('sp', xb[0:rows, :], x1scr[blk * 128:blk * 128 + rows, :])
                P.tt('dve', tmpf[0:rows, :], yacc(blk)[0:rows, :], Gt[0:rows, :], ALU.mult)
                P.tt('dve', x1b[0:rows, :], tmpf[0:rows, :], xb[0:rows, :], ALU.add)
                norm_rows(x1b[0:rows, :], rows, xnf[0:rows, :])
                P.tt('dve', tmpf[0:rows, 0:512], xnf[0:rows, 0:512], gfinA[0:rows, :], ALU.mult)
                P.tt('dve', tmpf[0:rows, 512:1024], xnf[0:rows, 512:1024], gfinB[0:rows, :], ALU.mult)
                if blk < 16:
                    P.dma('sp', yp[blk * 128:(blk + 1) * 128, :], tmpf[:, :])
                else:
                    P.dma('sp', ys, tmpf[0:32, :])

            slot = [0]

            def eload(src2d_rows, ncols, kparts):
                v = ESLOT[slot[0] % 4].rearrange("p (k n) -> p k n", k=kparts)
                slot[0] += 1
                for k in range(kparts):
                    P.dma('pool', v[:, k, :], src2d_rows(k))
                return v

            groups = [(0, 512, 4), (512, 512, 4), (1024, 512, 4), (1536, 512, 4), (2048, 32, 1)]
            hi = [0]
            for e in range(16):
                wg = eload(lambda k: w_gate[e, k * 128:(k + 1) * 128, :], 512, 8)
                wu = eload(lambda k: w_up[e, k * 128:(k + 1) * 128, :], 512, 8)
                wd = eload(lambda k: w_down[e, k * 128:(k + 1) * 128, :], 1024, 4)
                for (c0, N, nb) in groups:
                    hb = he[hi[0] % 2]
                    hi[0] += 1
                    for fc in range(4):
                        bg = nextbank()
                        bu = nextbank()
                        for dk in range(8):
                            P.mm(bg[:, 0:N], wg[:, dk, fc * 128:(fc + 1) * 128], H2T[:, dk, c0:c0 + N], start=(dk == 0), stop=(dk == 7))
                        for dk in range(8):
                            P.mm(bu[:, 0:N], wu[:, dk, fc * 128:(fc + 1) * 128], H2T[:, dk, c0:c0 + N], start=(dk == 0), stop=(dk == 7))
                        P.act(sil[:, 0:N], bg[:, 0:N], AF.Silu)
                        P.tt('dve', hb[:, fc, 0:N], sil[:, 0:N], bu[:, 0:N], ALU.mult)
                    for b in range(nb):
                        blk = c0 // 128 + b
                        rows = 128 if N == 512 else 32
                        for half in range(2):
                            by = nextbank()
                            for fc in range(4):
                                P.mm(by[0:rows, :], hb[:, fc, b * 128:b * 128 + rows], wd[:, fc, half * 512:(half + 1) * 512],
                                     start=(fc == 0), stop=(fc == 3))
                            ya = yacc(blk)[0:rows, half * 512:(half + 1) * 512]
                            if e == 0:
                                P.ts('dve', ya, by[0:rows, :], COMB[0:rows, blk, e:e + 1], None, op0=ALU.mult)
                            else:
                                P.stt('dve', ya, by[0:rows, :], COMB[0:rows, blk, e:e + 1], ya, ALU.mult, ALU.add)
                        if e == 15:
                            finalize(blk)

      except _Stop:
        pass
      P.emit()
    return nc, P


def router(P, LG, COMB, RT):
    lg = LG[:, :, 0:4]
    le = LG[:, :, 4:20].rearrange("p b (g j) -> p b g j", g=4)
    gmax = RT[:, :, 0:1]
    P.red('dve', RT[:, :, 0], lg, ALU.max)
    gsel = RT[:, :, 1:5]
    P.tt('dve', gsel, lg, gmax.to_broadcast([128, 17, 4]), ALU.is_equal)
    P.tt('dve', RT[:, :, 5:9], lg, gmax.to_broadcast([128, 17, 4]), ALU.subtract)
    P.act(RT[:, :, 5:9], RT[:, :, 5:9], AF.Exp)
    P.red('dve', RT[:, :, 9], RT[:, :, 5:9], ALU.add)
    P.recip(RT[:, :, 9], RT[:, :, 9])
    gp = RT[:, :, 9:10]
    sel = RT[:, :, 10:14]
    for g in range(4):
        if g == 0:
            P.tt('dve', sel, le[:, :, 0, :], gsel[:, :, 0:1].to_broadcast([128, 17, 4]), ALU.mult)
        else:
            P.tt('dve', RT[:, :, 5:9], le[:, :, g, :], gsel[:, :, g:g + 1].to_broadcast([128, 17, 4]), ALU.mult)
            P.tt('dve', sel, sel, RT[:, :, 5:9], ALU.add)
    m1 = RT[:, :, 14:15]
    P.red('dve', RT[:, :, 14], sel, ALU.max)
    mask1 = RT[:, :, 15:19]
    P.tt('dve', mask1, sel, m1.to_broadcast([128, 17, 4]), ALU.is_equal)
    sel2 = RT[:, :, 5:9]
    P.stt('dve', sel2, mask1, -1e30, sel, ALU.mult, ALU.add)
    m2 = RT[:, :, 19:20]
    P.red('dve', RT[:, :, 19], sel2, ALU.max)
    mask2 = RT[:, :, 20:24]
    P.tt('dve', mask2, sel2, m2.to_broadcast([128, 17, 4]), ALU.is_equal)
    d = RT[:, :, 0:1]
    P.tt('dve', d, m2, m1, ALU.subtract)
    P.act(d, d, AF.Exp)
    w1 = RT[:, :, 14:15]
    P.ts('dve', w1, d, 1.0, None, op0=ALU.add)
    P.recip(RT[:, :, 14], RT[:, :, 14])
    w2 = RT[:, :, 19:20]
    P.tt('dve', w2, d, w1, ALU.mult)
    P.tt('dve', w1, w1, gp, ALU.mult)
    P.tt('dve', w2, w2, gp, ALU.mult)
    cig = RT[:, :, 5:9]
    P.tt('dve', cig, mask1, w1.to_broadcast([128, 17, 4]), ALU.mult)
    P.tt('dve', mask2, mask2, w2.to_broadcast([128, 17, 4]), ALU.mult)
    P.tt('dve', cig, cig, mask2, ALU.add)
    cv = COMB.rearrange("p b (g j) -> p b g j", g=4) if False else None
    for g in range(4):
        P.tt('dve', COMB[:, :, g * 4:(g + 1) * 4], cig, gsel[:, :, g:g + 1].to_broadcast([128, 17, 4]), ALU.mult)


def sample_attention(P, nc, L):
    raise NotImplementedError


def _t5_bucket_np(n):
    n = np.maximum(n, 0)
    nf = np.maximum(n, 1).astype(np.float32)
    large = 16 + (np.log(nf / 16) / np.log(128 / 16) * 16).astype(np.int32)
    large = np.minimum(large, 31)
    return np.where(n < 16, n, large)


_CACHE = {}
DO_SAMPLE = True


def kernel(x_prompt, x_sample, c_prompt, c_sample, cache_k, cache_v, page_table,
           w_ada, b_ada, w_in, w_o, lam_q1, lam_k1, lam_q2, lam_k2, g_subln, rel_bias,
           g_sg_ln, b_sg_ln, w_s, b_s, w_rg, b_rg, w_re, b_re, w_gate, w_up, w_down, g_final):
    f32 = np.float32
    A = lambda a: np.ascontiguousarray(np.asarray(a))
    x_prompt = A(x_prompt); x_sample = A(x_sample)
    rel_bias = A(rel_bias)
    key = ('nc', DO_SAMPLE)
    if key not in _CACHE:
        _CACHE[key] = build_program(DO_SAMPLE)
    nc, P = _CACHE[key]

    kk = np.arange(128)[:, None]
    qq = np.arange(128)[None, :]
    dist_diag = qq - kk
    dist_sub = 128 + qq - kk
    dist_far = np.full((128, 128), 100000)
    bk_diag = _t5_bucket_np(dist_diag)
    bk_sub = _t5_bucket_np(dist_sub)
    bk_far = _t5_bucket_np(dist_far)
    m_diag = (dist_diag >= 0).astype(f32)
    m_one = np.ones((128, 128), f32)
    m_zero = np.zeros((128, 128), f32)
    tril = (np.arange(128)[:, None] <= np.arange(128)[None, :]).astype(f32)
    ii = np.arange(32)
    trilS = ((ii[:, None] // 8 == ii[None, :] // 8) & (ii[:, None] <= ii[None, :])).astype(f32)
    sel = np.zeros((5, 160), f32)
    sel[0, 0:128] = 1.0
    for s in range(4):
        sel[1 + s, 128 + s * 8:128 + (s + 1) * 8] = 1.0
    ident = np.eye(128).astype(ml_dtypes.bfloat16)
    identf = np.eye(128, dtype=f32)

    w_s0 = A(w_s)[0]
    wsT = np.ascontiguousarray(np.transpose(w_s0, (2, 0, 1)).reshape(128, 512))
    wsS = np.zeros((32, 4, 32), f32)
    for s in range(4):
        wsS[s * 8:(s + 1) * 8, :, s * 8:(s + 1) * 8] = np.transpose(w_s0[:, :8, :8], (2, 0, 1))
    wsS = wsS.reshape(32, 128)
    bs0 = A(b_s)[0]
    bsS = np.ascontiguousarray(bs0[:, np.arange(32) % 8]).reshape(1, 128)
    wr = np.ascontiguousarray(np.concatenate([A(w_rg)[0], A(w_re)[0]], axis=1))
    br = np.ascontiguousarray(np.concatenate([A(b_rg)[0], A(b_re)[0]])[None])
    lam4 = np.ascontiguousarray(np.concatenate([A(lam_q1)[0], A(lam_k1)[0], A(lam_q2)[0], A(lam_k2)[0]])[None])

    shared = dict(
        w_ada=A(w_ada)[0], b_ada=A(b_ada), w_in=A(w_in)[0], w_o=A(w_o)[0], lam4=lam4,
        gsub=np.ascontiguousarray(A(g_subln).reshape(128, 1)),
        b31=np.ascontiguousarray(rel_bias[31:32, :]),
        gsg=A(g_sg_ln).reshape(1, 512), bsg=A(b_sg_ln).reshape(1, 512),
        wsT=wsT, tril=tril, wsS=wsS, trilS=trilS, bs=bs0.reshape(1, 512), bsS=bsS,
        wr=wr, br=br, w_gate=A(w_gate)[0], w_up=A(w_up)[0], w_down=A(w_down)[0],
        gfin=A(g_final).reshape(1, D), ident=ident, identf=identf, sel=sel,
    )
    if DO_SAMPLE:
        shared['cache_k'] = A(cache_k).reshape(2560 * 128, 512)
        shared['cache_v'] = A(cache_v).reshape(2560 * 128, 512)
        pp = np.arange(128)
        hh = pp // 32
        q_of = (pp % 32) % 8
        cc = np.arange(136)
        kk_of = (np.minimum(cc, 127) % 32) * 4 + np.minimum(cc, 127) // 32
        dist_s = np.where(cc[None, :] < 128, 128 + q_of[:, None] - kk_of[None, :], q_of[:, None] - (cc[None, :] - 128))
        shared['sbias'] = np.ascontiguousarray(rel_bias[_t5_bucket_np(dist_s), hh[:, None]]).astype(f32)
        shared['smask'] = np.where(dist_s >= 0, 0.0, NEG).astype(f32)
        shared['sb31'] = np.ascontiguousarray(rel_bias[31, hh][:, None]).astype(f32)
        dsel = np.zeros((128, 64), f32)
        for h in range(4):
            for q in range(8):
                dsel[h * 32 + q, h * 8 + q] = 1.0
                dsel[h * 32 + 8 + q, 32 + h * 8 + q] = 1.0
        shared['dsel'] = dsel
        shared['pidx'] = (np.arange(128) % 32).astype(f32).reshape(128, 1)

    in_maps = []
    for c in range(NCORES):
        b, cpar = c // 2, c % 2
        order = []
        for g in range(8):
            order += [_own_block(cpar, 2 * g), _own_block(cpar, 2 * g + 1),
                      _partner_block(cpar, 2 * g), _partner_block(cpar, 2 * g + 1)]
        xb = x_prompt[b].reshape(32, 128, D)
        xpc = np.ascontiguousarray(xb[order].reshape(4096, D))
        cinc = np.ascontiguousarray(np.concatenate([A(c_prompt)[b:b + 1], A(c_sample)[4 * c:4 * c + 4]], axis=0))
        bks, mks = [bk_diag], [m_diag]
        for tau in (0, 1):
            second = (tau + cpar) % 2 == 1
            bks.append(bk_sub if second else bk_far)
            mks.append(m_one if second else m_zero)
        for tau in (0, 1):
            second = (tau + cpar) % 2 == 1
            bks.append(bk_far if second else bk_sub)
            mks.append(m_one)
        bg = np.stack([rel_bias[bk] for bk in bks], axis=0)
        bg = np.ascontiguousarray(np.transpose(bg, (1, 0, 3, 2)).reshape(128, 5 * 4 * 128)).astype(f32)
        mg = np.ascontiguousarray(np.transpose(np.stack(mks, 0), (1, 0, 2)).reshape(128, 5 * 128)).astype(f32)
        m = dict(shared)
        m.update(xp=xpc, xs=np.ascontiguousarray(x_sample[4 * c:4 * c + 4].reshape(32, D)), cin=cinc, biasg=bg, maskg=mg)
        if DO_SAMPLE:
            ptc = A(page_table)[4 * c:4 * c + 4].reshape(4, 16, 4)
            m['pt'] = np.ascontiguousarray(np.transpose(ptc, (2, 0, 1))[np.arange(128) // 32].reshape(128, 64)).astype(np.int32)
        in_maps.append(m)

    ncr = int(os.environ.get('MK_CORES', str(NCORES)))
    res = run_bass_kernel_spmd(nc, in_maps[:ncr], core_ids=list(range(ncr)))
    R = list(res.results) + [res.results[0]] * (NCORES - ncr)
    y_prompt = np.zeros((4, 4096, D), f32)
    kp = np.zeros((4, 1, 4096, 4, 128), f32)
    vp = np.zeros((4, 1, 4096, 4, 128), f32)
    y_sample = np.zeros((32, 8, D), f32)
    ks = np.zeros((32, 1, 8, 4, 128), f32)
    vs = np.zeros((32, 1, 8, 4, 128), f32)
    sg = np.zeros((32, 1, 8, 4, 128), f32)
    for c in range(NCORES):
        b, cpar = c // 2, c % 2
        r = R[c]
        for t in range(16):
            gblk = _own_block(cpar, t)
            y_prompt[b, gblk * 128:(gblk + 1) * 128] = r['yp'][t * 128:(t + 1) * 128]
            kp[b, 0, gblk * 128:(gblk + 1) * 128] = r['kp'][t * 128:(t + 1) * 128].reshape(128, 4, 128)
            vp[b, 0, gblk * 128:(gblk + 1) * 128] = r['vp'][t * 128:(t + 1) * 128].reshape(128, 4, 128)
        y_sample[4 * c:4 * c + 4] = r['ys'].reshape(4, 8, D)
        ks[4 * c:4 * c + 4, 0] = r['ks'].reshape(4, 8, 4, 128)
        vs[4 * c:4 * c + 4, 0] = r['vso'].reshape(4, 8, 4, 128)
        sg[4 * c:4 * c + 4, 0] = r['sgv'].reshape(4, 8, 4, 128)
    return (y_prompt, y_sample, kp, vp, ks, vs, sg)
```
